# Optimizing a Trainium2 kernel written in Bass

```python
import jax
import jax.numpy as jnp
from jax import lax
import numpy as np

D_MODEL = 4096
BATCH = 1
SEQ = 8192
DEPTH = 2

GRID_W = 64
CTX_LEN = 256
N_BRANCH = 4
BRANCH_WIDTH = D_MODEL // N_BRANCH
HEAD_DIM = 128
GM_CHUNK = 128
GM_GROUPS = BRANCH_WIDTH // HEAD_DIM
ATT_HEADS = BRANCH_WIDTH // HEAD_DIM
ATT_KV_HEADS = 2
ATT_REP = ATT_HEADS // ATT_KV_HEADS
KV_WIDTH = ATT_KV_HEADS * HEAD_DIM
ATT_Q_BLOCK = 128
ROPE_THETA = 10000.0
CONV_K = 31
ML_HEADS = BRANCH_WIDTH // HEAD_DIM
ML_HEAD_DIM = HEAD_DIM
ML_CHUNK = 128
ML_N_GATES = 4
N_EXPERTS = 16
EXPERT_FF = D_MODEL // 4
CAPACITY_FACTOR = 2
ALPHA = (2 * DEPTH) ** 0.25
BETA = (8 * DEPTH) ** -0.25
EPS = 1e-6
N_MOD = 6

PROJ_SPLITS = (
    ('gm_u', BRANCH_WIDTH), ('gm_v', BRANCH_WIDTH),
    ('att_q', BRANCH_WIDTH), ('att_k', KV_WIDTH), ('att_v', KV_WIDTH),
    ('cv_a', BRANCH_WIDTH), ('cv_b', BRANCH_WIDTH),
    ('ml_q', BRANCH_WIDTH), ('ml_k', BRANCH_WIDTH), ('ml_v', BRANCH_WIDTH), ('ml_o', BRANCH_WIDTH),
    ('ml_gates', ML_N_GATES * ML_HEADS),
    ('merge', N_BRANCH * D_MODEL),
)
IN_COLS = 9 * BRANCH_WIDTH + 2 * KV_WIDTH + ML_N_GATES * ML_HEADS + N_BRANCH * D_MODEL

kernel_name = 'hybrid_diffusion_trunk'


def _norm_stats(x):
    xf = x.astype(jnp.float32)
    xc = xf - jnp.mean(xf, axis=-1, keepdims=True)
    return xc * lax.rsqrt(jnp.mean(xc * xc, axis=-1, keepdims=True) + EPS)


def layer_norm(x, g, b):
    return (_norm_stats(x) * g.astype(jnp.float32) + b.astype(jnp.float32)).astype(x.dtype)


def rms_norm(x, g):
    xf = x.astype(jnp.float32)
    y = xf * lax.rsqrt(jnp.mean(xf * xf, axis=-1, keepdims=True) + EPS) * g.astype(jnp.float32)
    return y.astype(x.dtype)


def modulate(x, shift, scale):
    return x * (1.0 + scale) + shift


def split_projection(proj):
    out = {}
    start = 0
    for name, width in PROJ_SPLITS:
        out[name] = proj[..., start:start + width]
        start += width
    return out


def axial_rope_tables(n_tokens):
    rows = n_tokens // GRID_W
    row = jnp.repeat(jnp.arange(rows), GRID_W).astype(jnp.float32)
    col = (jnp.arange(n_tokens) % GRID_W).astype(jnp.float32)
    n_freq = HEAD_DIM // 4
    inv = ROPE_THETA ** (-jnp.arange(n_freq, dtype=jnp.float32) / n_freq)
    ang_r = row[:, None] * inv[None, :]
    ang_c = col[:, None] * inv[None, :]
    return (jnp.cos(ang_r), jnp.sin(ang_r), jnp.cos(ang_c), jnp.sin(ang_c))


def apply_axial_rope(x, rope):
    cos_r, sin_r, cos_c, sin_c = rope

    def rotate(xh, cos, sin):
        x1, x2 = jnp.split(xh, 2, axis=-1)
        cos = cos[None, :, None, :]
        sin = sin[None, :, None, :]
        return jnp.concatenate([x1 * cos - x2 * sin, x2 * cos + x1 * sin], axis=-1)

    x_row, x_col = jnp.split(x.astype(jnp.float32), 2, axis=-1)
    out = jnp.concatenate([rotate(x_row, cos_r, sin_r), rotate(x_col, cos_c, sin_c)], axis=-1)
    return out.astype(x.dtype)


def chunk_gmlp(P, p):
    B, T, _ = P['gm_u'].shape
    u = jax.nn.gelu(P['gm_u'])
    v = layer_norm(jax.nn.gelu(P['gm_v']), p['gm_ln_g'], p['gm_ln_b'])
    v = v.reshape(B, T // GM_CHUNK, GM_CHUNK, GM_GROUPS, BRANCH_WIDTH // GM_GROUPS)
    s = jnp.einsum('gpq,bnqgc->bnpgc', p['gm_w_s'].astype(v.dtype), v)
    s = s + p['gm_b_s'].T.astype(v.dtype)[None, None, :, :, None]
    return u * s.reshape(B, T, BRANCH_WIDTH)


def attn_heads(P, p, rope):
    B, T, _ = P['att_q'].shape
    q = rms_norm(P['att_q'].reshape(B, T, ATT_HEADS, HEAD_DIM), p['att_q_gain'])
    k = rms_norm(P['att_k'].reshape(B, T, ATT_KV_HEADS, HEAD_DIM), p['att_k_gain'])
    v = P['att_v'].reshape(B, T, ATT_KV_HEADS, HEAD_DIM)
    if rope is not None:
        q = apply_axial_rope(q, rope)
        k = apply_axial_rope(k, rope)
    return q, k, v


def gqa_attend(q, k, v):
    B, T, _, _ = q.shape
    n_blocks = T // ATT_Q_BLOCK
    qb = jnp.moveaxis(q.reshape(B, n_blocks, ATT_Q_BLOCK, ATT_KV_HEADS, ATT_REP, HEAD_DIM), 1, 0)
    scale = HEAD_DIM ** -0.5

    def one_block(q_blk):
        s = jnp.einsum('bqgrd,bkgd->bgrqk', q_blk, k).astype(jnp.float32) * scale
        w = jax.nn.softmax(s, axis=-1).astype(v.dtype)
        return jnp.einsum('bgrqk,bkgd->bqgrd', w, v)

    o = lax.map(one_block, qb)
    return jnp.moveaxis(o, 0, 1).reshape(B, T, ATT_HEADS * HEAD_DIM)


def conformer_conv(P, p):
    g = P['cv_a'] * jax.nn.sigmoid(P['cv_b'])
    w = p['conv_w'][:, None, :].astype(g.dtype)
    y = lax.conv_general_dilated(g, w, window_strides=(1,), padding=[(CONV_K // 2, CONV_K // 2)],
                                 dimension_numbers=('NWC', 'WIO', 'NWC'),
                                 feature_group_count=BRANCH_WIDTH)
    y = y + p['conv_b'].astype(y.dtype)
    return jax.nn.silu(layer_norm(y, p['conv_ln_g'], p['conv_ln_b']))


def mlstm_prep(P, gate_bias):
    B, T, _ = P['ml_q'].shape

    def heads(a):
        return jnp.swapaxes(a.reshape(B, T, ML_HEADS, ML_HEAD_DIM), 1, 2).astype(jnp.float32)

    q = heads(P['ml_q'])
    k = heads(P['ml_k']) * (ML_HEAD_DIM ** -0.5)
    v = heads(P['ml_v'])
    g = P['ml_gates'].reshape(B, T, ML_N_GATES, ML_HEADS).astype(jnp.float32) + gate_bias.astype(jnp.float32)
    g = jnp.transpose(g, (2, 0, 3, 1))
    fw = (g[0], jax.nn.log_sigmoid(g[1]))
    bw = (g[2], jax.nn.log_sigmoid(g[3]))
    return q, k, v, fw, bw


def mlstm_zero_state(batch):
    return (jnp.zeros((batch, ML_HEADS, ML_HEAD_DIM, ML_HEAD_DIM), jnp.float32),
            jnp.zeros((batch, ML_HEADS, ML_HEAD_DIM), jnp.float32),
            jnp.zeros((batch, ML_HEADS), jnp.float32))


def mlstm_chunk_scan(q, k, v, log_i, log_f, state):
    B, H, T, d = q.shape
    nc = T // ML_CHUNK

    def chunks(a):
        return jnp.moveaxis(a.reshape(B, H, nc, ML_CHUNK, *a.shape[3:]), 2, 0)

    lower = jnp.tril(jnp.ones((ML_CHUNK, ML_CHUNK), dtype=bool))

    def step(carry, inp):
        C, n, m = carry
        qc, kc, vc, ic, fc = inp
        b = jnp.cumsum(fc, axis=-1)
        inter = b + m[..., None]
        dlog = b[..., :, None] - b[..., None, :] + ic[..., None, :]
        dlog = jnp.where(lower, dlog, -jnp.inf)
        mj = jnp.maximum(inter, jnp.max(dlog, axis=-1))
        w_inter = jnp.exp(inter - mj)
        s = jnp.einsum('bhjd,bhsd->bhjs', qc, kc) * jnp.exp(dlog - mj[..., None])
        num = (w_inter[..., None] * jnp.einsum('bhjd,bhde->bhje', qc, C)
               + jnp.einsum('bhjs,bhse->bhje', s, vc))
        den = w_inter * jnp.einsum('bhjd,bhd->bhj', qc, n) + jnp.sum(s, axis=-1)
        h = num / jnp.maximum(jnp.abs(den), jnp.exp(-mj))[..., None]
        m_new = mj[..., -1]
        w_c = jnp.exp(b[..., -1] + m - m_new)
        w_s = jnp.exp(b[..., -1:] - b + ic - m_new[..., None])
        C_new = w_c[..., None, None] * C + jnp.einsum('bhs,bhsd,bhse->bhde', w_s, kc, vc)
        n_new = w_c[..., None] * n + jnp.einsum('bhs,bhsd->bhd', w_s, kc)
        return (C_new, n_new, m_new), h

    final, h = lax.scan(step, state, (chunks(q), chunks(k), chunks(v), chunks(log_i), chunks(log_f)))
    return jnp.moveaxis(h, 0, 2).reshape(B, H, T, d), final


def mlstm_bidirectional(q, k, v, fw, bw, state_f, state_b):
    h_f, fin_f = mlstm_chunk_scan(q, k, v, fw[0], fw[1], state_f)

    def rev(a):
        return jnp.flip(a, axis=2)

    h_b, fin_b = mlstm_chunk_scan(rev(q), rev(k), rev(v), rev(bw[0]), rev(bw[1]), state_b)
    return h_f + rev(h_b), fin_f, fin_b


def mlstm_out(h, o_pre, norm_g):
    B, H, T, d = h.shape
    hn = _norm_stats(jnp.swapaxes(h, 1, 2)) * norm_g.astype(jnp.float32).reshape(H, d)
    return jax.nn.sigmoid(o_pre) * hn.reshape(B, T, H * d).astype(o_pre.dtype)


def merge_branches(gate_pre, branches, w_branch, w_out):
    B, T, _ = gate_pre.shape
    gates = jax.nn.sigmoid(gate_pre.reshape(B, T, N_BRANCH, D_MODEL))
    y = gates[:, :, 0] * (branches[0] @ w_branch[0])
    for i in range(1, N_BRANCH):
        y = y + gates[:, :, i] * (branches[i] @ w_branch[i])
    return y @ w_out


def token_mixer(h_lat, h_ctx, p, rope, need_ctx_out):
    P_lat = split_projection(h_lat @ p['w_in'])
    P_ctx = split_projection(h_ctx @ p['w_in'])
    q_l, k_l, v_l = attn_heads(P_lat, p, rope)
    q_c, k_c, v_c = attn_heads(P_ctx, p, None)
    att_lat = gqa_attend(q_l, jnp.concatenate([k_l, k_c], axis=1), jnp.concatenate([v_l, v_c], axis=1))
    zero = mlstm_zero_state(h_ctx.shape[0])
    mq_c, mk_c, mv_c, fw_c, bw_c = mlstm_prep(P_ctx, p['ml_gate_bias'])
    h_c, st_f, st_b = mlstm_bidirectional(mq_c, mk_c, mv_c, fw_c, bw_c, zero, zero)
    mq_l, mk_l, mv_l, fw_l, bw_l = mlstm_prep(P_lat, p['ml_gate_bias'])
    h_l, _, _ = mlstm_bidirectional(mq_l, mk_l, mv_l, fw_l, bw_l, st_f, st_b)
    y_lat = merge_branches(
        P_lat['merge'],
        [chunk_gmlp(P_lat, p), att_lat, conformer_conv(P_lat, p), mlstm_out(h_l, P_lat['ml_o'], p['ml_norm_g'])],
        p['w_branch'], p['w_out'])
    if not need_ctx_out:
        return y_lat, None
    att_ctx = gqa_attend(q_c, k_c, v_c)
    y_ctx = merge_branches(
        P_ctx['merge'],
        [chunk_gmlp(P_ctx, p), att_ctx, conformer_conv(P_ctx, p), mlstm_out(h_c, P_ctx['ml_o'], p['ml_norm_g'])],
        p['w_branch'], p['w_out'])
    return y_lat, y_ctx


def expert_choice_ffn(h, w_router, w_gate, w_up, w_down):
    B, T, D = h.shape
    cap = CAPACITY_FACTOR * T // N_EXPERTS
    aff = jax.nn.softmax(jnp.einsum('btd,de->bte', h, w_router).astype(jnp.float32), axis=-1)
    g, idx = lax.top_k(jnp.swapaxes(aff, 1, 2), cap)
    xe = jax.vmap(lambda hb, ib: hb[ib])(h, idx)
    hid = jax.nn.silu(jnp.einsum('becd,edf->becf', xe, w_gate)) * jnp.einsum('becd,edf->becf', xe, w_up)
    ye = jnp.einsum('becf,efd->becd', hid, w_down) * g[..., None].astype(h.dtype)

    def combine(ib, yb):
        return jnp.zeros((T, D), yb.dtype).at[ib.reshape(-1)].add(yb.reshape(-1, D))

    return jax.vmap(combine)(idx, ye)


def setup_inputs(seed: int = 0) -> dict:
    key = jax.random.key(seed)
    keys = iter(jax.random.split(key, 32))

    def nrm(shape, scale):
        return jax.random.normal(next(keys), shape, jnp.float32) * scale

    def gain(shape):
        return 1.0 + nrm(shape, 0.01)

    L, D, BW, E, F = DEPTH, D_MODEL, BRANCH_WIDTH, N_EXPERTS, EXPERT_FF
    ml_gate_bias = nrm((L, ML_N_GATES, ML_HEADS), 0.1).at[:, 1::2].add(3.0)
    return {
        'x': nrm((BATCH, SEQ, D), 1.0),
        'c': nrm((BATCH, D), 1.0),
        'ctx': nrm((BATCH, CTX_LEN, D), 1.0),
        'c_ctx': nrm((D,), 1.0),
        'w_mod': nrm((L, D, N_MOD * D), 0.5 * D ** -0.5),
        'b_mod': nrm((L, N_MOD * D), 0.01),
        'w_in': nrm((L, D, IN_COLS), D ** -0.5),
        'att_q_gain': gain((L, HEAD_DIM)),
        'att_k_gain': gain((L, HEAD_DIM)),
        'gm_ln_g': gain((L, BW)),
        'gm_ln_b': nrm((L, BW), 0.01),
        'gm_w_s': nrm((L, GM_GROUPS, GM_CHUNK, GM_CHUNK), 0.5 * GM_CHUNK ** -0.5),
        'gm_b_s': gain((L, GM_GROUPS, GM_CHUNK)),
        'conv_w': nrm((L, CONV_K, BW), CONV_K ** -0.5),
        'conv_b': nrm((L, BW), 0.01),
        'conv_ln_g': gain((L, BW)),
        'conv_ln_b': nrm((L, BW), 0.01),
        'ml_gate_bias': ml_gate_bias,
        'ml_norm_g': gain((L, BW)),
        'w_branch': nrm((L, N_BRANCH, BW, D), BETA * BW ** -0.5),
        'w_out': nrm((L, D, D), BETA * D ** -0.5),
        'ln1_g': gain((L, D)),
        'ln1_b': nrm((L, D), 0.01),
        'w_router': nrm((L, D, E), D ** -0.5),
        'w_gate': nrm((L, E, D, F), D ** -0.5),
        'w_up': nrm((L, E, D, F), D ** -0.5),
        'w_down': nrm((L, E, F, D), BETA * F ** -0.5),
        'ln2_g': gain((L, D)),
        'ln2_b': nrm((L, D), 0.01),
    }


def reference(x, c, ctx, c_ctx, w_mod, b_mod, w_in, att_q_gain, att_k_gain, gm_ln_g, gm_ln_b,
              gm_w_s, gm_b_s, conv_w, conv_b, conv_ln_g, conv_ln_b, ml_gate_bias, ml_norm_g,
              w_branch, w_out, ln1_g, ln1_b, w_router, w_gate, w_up, w_down, ln2_g, ln2_b):
    rope = axial_rope_tables(x.shape[1])
    xc = ctx
    for l in range(DEPTH):
        last = l == DEPTH - 1
        mod_lat = jnp.split((jax.nn.silu(c) @ w_mod[l] + b_mod[l])[:, None, :], N_MOD, axis=-1)
        mod_ctx = jnp.split((jax.nn.silu(c_ctx) @ w_mod[l] + b_mod[l])[None, None, :], N_MOD, axis=-1)
        p = {
            'w_in': w_in[l], 'att_q_gain': att_q_gain[l], 'att_k_gain': att_k_gain[l],
            'gm_ln_g': gm_ln_g[l], 'gm_ln_b': gm_ln_b[l], 'gm_w_s': gm_w_s[l], 'gm_b_s': gm_b_s[l],
            'conv_w': conv_w[l], 'conv_b': conv_b[l], 'conv_ln_g': conv_ln_g[l], 'conv_ln_b': conv_ln_b[l],
            'ml_gate_bias': ml_gate_bias[l], 'ml_norm_g': ml_norm_g[l],
            'w_branch': w_branch[l], 'w_out': w_out[l],
        }
        y_lat, y_ctx = token_mixer(modulate(x, mod_lat[0], mod_lat[1]),
                                   modulate(xc, mod_ctx[0], mod_ctx[1]), p, rope, not last)
        x = layer_norm(ALPHA * x + mod_lat[2] * y_lat, ln1_g[l], ln1_b[l])
        y_lat = expert_choice_ffn(modulate(x, mod_lat[3], mod_lat[4]), w_router[l], w_gate[l], w_up[l], w_down[l])
        x = layer_norm(ALPHA * x + mod_lat[5] * y_lat, ln2_g[l], ln2_b[l])
        if not last:
            xc = layer_norm(ALPHA * xc + mod_ctx[2] * y_ctx, ln1_g[l], ln1_b[l])
            y_c = expert_choice_ffn(modulate(xc, mod_ctx[3], mod_ctx[4]), w_router[l], w_gate[l], w_up[l], w_down[l])
            xc = layer_norm(ALPHA * xc + mod_ctx[5] * y_c, ln2_g[l], ln2_b[l])
    return x
```

```python
import numpy as np
from contextlib import ExitStack
import concourse.bass as bass
import concourse.mybir as mybir
from concourse.bass_utils import run_bass_kernel_spmd

F32 = mybir.dt.float32
BF16 = mybir.dt.bfloat16
I32 = mybir.dt.int32
AF = mybir.ActivationFunctionType
ALU = mybir.AluOpType
AX = mybir.AxisListType

D = 4096
KC = 32
BW = 1024
NEXP = 16
FF = 1024
EPS = 1e-6
OFF = dict(gm_u=0, gm_v=1024, att_q=2048, att_k=3072, att_v=3328, cv_a=3584, cv_b=4608,
           ml_q=5632, ml_k=6656, ml_v=7680, ml_o=8704, ml_g=9728, merge=9760)


class KB:
    def __init__(self, nc, stack):
        self.nc = nc
        self.stack = stack
        self.eng = {'pe': nc.tensor, 'act': nc.scalar, 'dve': nc.vector,
                    'pool': nc.gpsimd, 'sp': nc.sync}
        self.esem = {}
        self.ecnt = {}
        for n in ['pe', 'act', 'dve', 'pool']:
            self.esem[n] = stack.enter_context(nc.semaphore('es_' + n))
            self.ecnt[n] = 0
        self.seen = {n: {} for n in self.eng}
        self.lastw = {}
        self.readers = {}
        self.dsem = {}
        self.dcnt = {}
        self.ninst = 0
        self.free = []
        self.nsem = 0

    def _wait(self, e, evs):
        need = {}
        for ev in evs:
            if ev is None:
                continue
            s, v = ev
            k = id(s)
            if e == 'pe' and s is self.esem['pe']:
                continue
            if self.seen[e].get(k, 0) >= v:
                continue
            if k not in need or need[k][1] < v:
                need[k] = (s, v)
        for k, (s, v) in need.items():
            self.eng[e].wait_ge(s, v)
            self.seen[e][k] = v

    def _deps(self, reads, writes):
        evs = []
        for r in reads:
            evs.append(self.lastw.get(r))
        for w in writes:
            evs.append(self.lastw.get(w))
            evs.extend(self.readers.get(w, ()))
        return evs

    def _commit(self, ev, reads, writes):
        for r in reads:
            self.readers.setdefault(r, []).append(ev)
        for w in writes:
            self.lastw[w] = ev
            self.readers[w] = []

    @staticmethod
    def _is_psum(r):
        return r == 'psb' or (isinstance(r, tuple) and len(r) == 2 and r[0] == 'ps')

    def op(self, e, fn, reads=(), writes=()):
        psr = [r for r in reads if self._is_psum(r)]
        if psr:
            writes = list(writes) + [r for r in psr if r not in writes]
        self._wait(e, self._deps(reads, writes))
        inst = fn()
        self.ecnt[e] += 1
        inst.then_inc(self.esem[e], 1)
        ev = (self.esem[e], self.ecnt[e])
        self._commit(ev, reads, writes)
        self.ninst += 1
        return ev

    def dma(self, q, out, in_, reads=(), writes=(), key=None, indirect=None, **kw):
        if key is None:
            key = writes[0] if writes else ('st', reads[0])
        if key not in self.dsem:
            if self.free:
                self.dsem[key], self.dcnt[key] = self.free.pop()
            else:
                self.nsem += 1
                self.dsem[key] = self.stack.enter_context(self.nc.semaphore('ds%d' % self.nsem))
                self.dcnt[key] = 0
        self._wait(q, self._deps(reads, writes))
        if indirect is not None:
            inst = self.nc.gpsimd.indirect_dma_start(out=out, in_=in_, **indirect)
        else:
            inst = self.eng[q].dma_start(out=out, in_=in_, **kw)
        self.dcnt[key] += 16
        inst.then_inc(self.dsem[key], 16)
        ev = (self.dsem[key], self.dcnt[key])
        self._commit(ev, reads, writes)
        self.ninst += 1
        return ev

    def barrier(self):
        evs = [(self.esem[n], self.ecnt[n]) for n in self.esem if self.ecnt[n] > 0]
        evs += [(self.dsem[k], self.dcnt[k]) for k in self.dsem]
        for e in ['pe', 'act', 'dve', 'pool', 'sp']:
            self._wait(e, evs)
        self.lastw = {}
        self.readers = {}
        for k in self.dsem:
            self.free.append((self.dsem[k], self.dcnt[k]))
        self.dsem = {}
        self.dcnt = {}


_UID = [0]


def sbt(nc, name, shape, dt):
    _UID[0] += 1
    return nc.sbuf_tensor('%s_u%d' % (name, _UID[0]), list(shape), dt)


class Rot:
    def __init__(self, nc, st, name, shape, dt, n):
        self.t = [st.enter_context(sbt(nc, '%s%d' % (name, i), list(shape), dt)) for i in range(n)]
        self.name = name
        self.i = 0

    def nxt(self):
        k = self.i % len(self.t)
        self.i += 1
        return self.t[k], (self.name, k)


def pipeline(n, load, comp, depth=1):
    for i in range(min(depth, n)):
        load(i)
    for i in range(n):
        if i + depth < n:
            load(i + depth)
        comp(i)


def build(SEQ, CTX, DEPTH, dbg=(), upto=None, proj_sel=None, small_moe=False):
    T = SEQ + CTX
    NCH = T // 128
    nc = bass.Bass("TRN2", target_bir_lowering=False)
    st0 = ExitStack()

    def din(name, shape, dt=F32):
        return nc.dram_tensor(name, list(shape), dt, kind="ExternalInput").ap()

    def dscr(name, shape, dt):
        kind = "ExternalOutput" if name in dbg else "Internal"
        return nc.dram_tensor(name, list(shape), dt, kind=kind).ap()

    L = DEPTH
    x_in = din('x', [SEQ, D]); ctx_in = din('ctx', [CTX, D])
    cc_in = din('cc', [128, KC, 2])
    w_mod = din('w_mod', [L, D, 6 * D]); b_modc = din('b_modc', [L, 128, 192])
    w_in = din('w_in', [L, D, 26144])
    aq_gain = din('att_q_gain', [L, 128]); ak_gain = din('att_k_gain', [L, 128])
    gm_ln_g = din('gm_ln_g', [L, BW]); gm_ln_b = din('gm_ln_b', [L, BW])
    gm_wsT = din('gm_wsT', [L, 8, 128, 128]); gm_b_s = din('gm_b_s', [L, 8 * 128])
    conv_wT = din('conv_wT', [L, BW, 31]); conv_bc = din('conv_bc', [L, 128, 8])
    conv_ln_gc = din('conv_ln_gc', [L, 128, 8]); conv_ln_bc = din('conv_ln_bc', [L, 128, 8])
    ml_gate_bias = din('ml_gate_bias', [L, 32]); ml_norm_gc = din('ml_norm_gc', [L, 128, 8])
    w_branch = din('w_branch', [L, 4, BW, D]); w_out = din('w_out', [L, D, D])
    ln1_g = din('ln1_g', [L, D]); ln1_b = din('ln1_b', [L, D])
    w_router = din('w_router', [L, D, NEXP])
    NE_DECL = 1 if small_moe else NEXP
    w_gate = din('w_gate', [L, NE_DECL, D, FF]); w_up = din('w_up', [L, NE_DECL, D, FF])
    w_down = din('w_down', [L, NE_DECL, FF, D])
    ln2_g = din('ln2_g', [L, D]); ln2_b = din('ln2_b', [L, D])
    cst = din('cst', [128, 8, 128])
    ropeC = din('ropeC', [SEQ, 64]); ropeS = din('ropeS', [SEQ, 64])
    iota_in = din('iota', [128, 1024]); tid_in = din('tid', [128, 64])
    zeros_in = din('zeros', [128, 4096])
    out_d = nc.dram_tensor('out', [SEQ, D], F32, kind="ExternalOutput").ap()

    XA = dscr('XA', [T, D], F32)
    NBLK = SEQ // 512 + (CTX + 511) // 512
    HT = dscr('HT', [NBLK, 128, KC, 512], BF16)
    GU = dscr('GU', [BW, T], BF16); GV = dscr('GV', [T, BW], F32)
    AQ = dscr('AQ', [T, BW], F32); AK = dscr('AK', [T, 256], F32); AV = dscr('AV', [T, 256], BF16)
    QT = dscr('QT', [BW, T], BF16); KT = dscr('KT', [256, T], BF16)
    CG = dscr('CG', [BW, T], F32)
    MQ = dscr('MQ', [BW, T], BF16); MK = dscr('MK', [BW, T], BF16); MKt = dscr('MKt', [T, BW], BF16)
    MV = dscr('MV', [T, BW], BF16); MO = dscr('MO', [BW, T], BF16); GT = dscr('GT', [T, 32], F32)
    BR = dscr('BR', [4, BW, T], BF16)
    HF = dscr('HF', [BW, T], F32)
    YT = dscr('YT', [NBLK, 128, KC, 512], BF16)
    R1 = dscr('R1', [T, D], F32)
    XM = dscr('XM', [T, D], BF16)
    YM = [dscr('YM%d' % c_, [T, 512], F32) for c_ in range(8)]
    MODV = dscr('MODV', [L, 2, 6 * D], F32)

    kb = KB(nc, st0)
    PS = [st0.enter_context(nc.psum_tensor('ps%d' % i, [128, 512], F32)) for i in range(7)]
    PSB = st0.enter_context(nc.psum_tensor('psb', [128, 1024], BF16))
    psi = [0]
    bankset = [[0, 1, 2, 3, 4, 5, 6]]
    ALPHA = float((2 * 2) ** 0.25)

    def bank():
        k = bankset[0][psi[0] % len(bankset[0])]
        psi[0] += 1
        return PS[k], ('ps', k)

    CST = st0.enter_context(sbt(nc, 'CST', [128, 8, 128], F32))
    IDB = st0.enter_context(sbt(nc, 'IDB', [128, 128], BF16))
    ONEB = st0.enter_context(sbt(nc, 'ONEB', [128, 128], BF16))
    MODC = st0.enter_context(sbt(nc, 'MODC', [128, L, 192, 2], F32))
    MOD1P = st0.enter_context(sbt(nc, 'MOD1P', [128, L, 64, 2], F32))
    EPSC = st0.enter_context(sbt(nc, 'EPSC', [128, 1], F32))
    ONEC = st0.enter_context(sbt(nc, 'ONEC', [128, 1], F32))
    kb.op('pool', lambda: nc.gpsimd.memset(EPSC[:], EPS), [], ['EPSC'])
    kb.op('pool', lambda: nc.gpsimd.memset(ONEC[:], 1.0), [], ['ONEC'])
    kb.dma('sp', CST[:], cst, writes=['CST'])
    IDF = CST[:, 0, :]; ULE = CST[:, 1, :]; UGE = CST[:, 2, :]; ONEF = CST[:, 3, :]
    MLE = CST[:, 4, :]; MGE = CST[:, 5, :]; USTR = CST[:, 6, :]
    kb.op('dve', lambda: nc.vector.tensor_copy(out=IDB[:], in_=IDF), reads=['CST'], writes=['IDB'])
    kb.op('dve', lambda: nc.vector.tensor_copy(out=ONEB[:], in_=ONEF), reads=['CST'], writes=['ONEB'])

    streams = [('lat', 0, SEQ, 0), ('ctx', SEQ, CTX, 1)]
    blocks = []
    for (sn, u0, n, si) in streams:
        for b0 in range(0, n, 512):
            blocks.append((u0 + b0, min(512, n - b0), si))
    chunks = [(u, si) for (sn, u0, n, si) in streams for u in range(u0, u0 + n, 128)]

    def blk_of(u):
        if u < SEQ:
            return u // 512, u % 512
        return SEQ // 512 + (u - SEQ) // 512, (u - SEQ) % 512

    def act(out, in_, func, reads, writes, **kw):
        return kb.op('act', lambda: nc.scalar.activation(out=out, in_=in_, func=func, **kw), reads, writes)

    def tt(out, a, b, op, reads, writes, e='dve'):
        eng = nc.vector if e == 'dve' else nc.gpsimd
        return kb.op(e, lambda: eng.tensor_tensor(out=out, in0=a, in1=b, op=op), reads, writes)

    def ts(out, a, s1, s2, op0, op1, reads, writes, e='dve'):
        eng = nc.vector if e == 'dve' else nc.gpsimd
        if op1 is None:
            return kb.op(e, lambda: eng.tensor_scalar(out=out, in0=a, scalar1=s1, scalar2=None, op0=op0), reads, writes)
        return kb.op(e, lambda: eng.tensor_scalar(out=out, in0=a, scalar1=s1, scalar2=s2, op0=op0, op1=op1), reads, writes)

    def stt(out, a, s, b, op0, op1, reads, writes):
        return kb.op('dve', lambda: nc.vector.scalar_tensor_tensor(out=out, in0=a, scalar=s, in1=b, op0=op0, op1=op1), reads, writes)

    def cp(out, in_, reads, writes, e='dve'):
        eng = nc.vector if e == 'dve' else nc.gpsimd
        return kb.op(e, lambda: eng.tensor_copy(out=out, in_=in_), reads, writes)

    def mm(out, lhsT, rhs, start, stop, reads, writes):
        return kb.op('pe', lambda: nc.tensor.matmul(out, lhsT=lhsT, rhs=rhs, start=start, stop=stop), reads, writes)

    castsel = [0]

    def cast_load(dst3, src2, nk, res, stg):
        n = dst3.shape[-1]
        SE = stg.t[0].shape[1]
        kk = max(1, min(nk, SE // n))
        e = 'pool' if castsel[0] % 2 == 0 else 'act'
        castsel[0] += 1
        for k0 in range(0, nk, kk):
            k1 = min(nk, k0 + kk)
            s_t, s_r = stg.nxt()
            sv = s_t[:, 0:(k1 - k0) * n].rearrange('p (k n) -> p k n', n=n)
            kb.dma('sp', sv, src2[k0 * 128:k1 * 128, :].rearrange('(k p) n -> p k n', p=128), writes=[s_r])
            if e == 'pool':
                cp(dst3[:, k0:k1, :], sv, [s_r], [res], e='dve')
            else:
                act(dst3[:, k0:k1, :], sv, AF.Copy, [s_r], [res])

    def tr(out, in_, ident, reads, writes):
        return kb.op('pe', lambda: nc.tensor.transpose(out, in_, ident), reads, writes)

    for r0_ in range(0, SEQ, 512):
        kb.dma('sp', XA[r0_:r0_ + 512, :], x_in[r0_:r0_ + 512, :], key='cpx')
    kb.dma('sp', XA[SEQ:T, :], ctx_in, key='cpx')
    with ExitStack() as ph:
        sc = ph.enter_context(sbt(nc, 'sc', [128, KC, 2], F32))
        bm = ph.enter_context(sbt(nc, 'bm', [128, 192], F32))
        wr = Rot(nc, ph, 'wmod', [128, KC, 512], F32, 2)
        kb.dma('sp', sc[:], cc_in, writes=['sc'])
        act(sc[:], sc[:], AF.Silu, ['sc'], ['sc'])
        for l in range(L):
            kb.dma('sp', bm[:], b_modc[l], writes=['bm'])
            tiles = {}

            def ld(i, l=l):
                t, r = wr.nxt()
                tiles[i] = (t, r)
                kb.dma('sp', t[:], w_mod[l][:, i * 512:(i + 1) * 512].rearrange('(kc p) n -> p kc n', p=128), writes=[r])

            def cmp_(i, l=l):
                t, r = tiles.pop(i)
                for fc in range(4):
                    j = i * 4 + fc
                    p, pr = bank()
                    for kc in range(KC):
                        mm(p[:, 0:2], t[:, kc, fc * 128:(fc + 1) * 128], sc[:, kc, :], kc == 0, kc == KC - 1, [r, 'sc'], [pr])
                    ts(MODC[:, l, j, :], p[:, 0:2], bm[:, j:j + 1], None, ALU.add, None, [pr, 'bm'], ['MODC'])
            pipeline(48, ld, cmp_)
            ts(MOD1P[:, l, 0:32, :], MODC[:, l, 32:64, :], 1.0, None, ALU.add, None, ['MODC'], ['MOD1P'])
            ts(MOD1P[:, l, 32:64, :], MODC[:, l, 128:160, :], 1.0, None, ALU.add, None, ['MODC'], ['MOD1P'])
            for s in range(2):
                for (j0, nj) in [(0, 128), (128, 64)]:
                    tmp = ph.enter_context(sbt(nc, 'mt%d_%d_%d' % (l, s, j0), [128, 128], F32))
                    tmo = ph.enter_context(sbt(nc, 'mo%d_%d_%d' % (l, s, j0), [128, 128], F32))
                    rk = ('mt', l, s, j0)
                    cp(tmp[:, 0:nj], MODC[:, l, j0:j0 + nj, s], ['MODC'], [rk])
                    p, pr = bank()
                    tr(p[0:nj, 0:128], tmp[:, 0:nj], IDF, [rk, 'CST'], [pr])
                    cp(tmo[0:nj, :], p[0:nj, 0:128], [pr], [(rk, 'o')])
                    kb.dma('sp', MODV[l, s, j0 * 128:(j0 + nj) * 128].rearrange('(j p) -> j p', p=128), tmo[0:nj, :], reads=[(rk, 'o')])
    kb.barrier()

    def modcol(l, m, si):
        return MODC[:, l, m * 32:(m + 1) * 32, si]

    def gelu_tanh(out, src, srcres, outres, scr, w):
        s1, r1 = scr.nxt()
        act(s1[:, 0:w], src, AF.Square, srcres, [r1])
        ts(s1[:, 0:w], s1[:, 0:w], 0.044715, 1.0, ALU.mult, ALU.add, [r1], [r1])
        tt(s1[:, 0:w], s1[:, 0:w], src, ALU.mult, [r1] + srcres, [r1])
        act(s1[:, 0:w], s1[:, 0:w], AF.Sigmoid, [r1], [r1], scale=1.5957691216)
        tt(out, s1[:, 0:w], src, ALU.mult, [r1] + srcres, outres)

    def layer(l):
        last = (l == L - 1)
        with ExitStack() as ph:
            xr = Rot(nc, ph, 'xin', [128, D], F32, 2)
            hr = Rot(nc, ph, 'hto', [128, KC, 128], BF16, 2)
            tl = {}

            def ld(i):
                t, r = xr.nxt(); tl[i] = (t, r)
                u, si = chunks[i]
                kb.dma('sp', t[:], XA[u:u + 128, :], writes=[r])

            def cmp_(i):
                t, r = tl.pop(i)
                u, si = chunks[i]
                ho, hres = hr.nxt()
                for q in range(8):
                    p, pr = bank()
                    for k4 in range(4):
                        kc = q * 4 + k4
                        tr(p[:, k4 * 128:(k4 + 1) * 128], t[:, kc * 128:(kc + 1) * 128], IDF, [r, 'CST'], [pr])
                    for k4 in range(4):
                        kc = q * 4 + k4
                        act(ho[:, kc, :], p[:, k4 * 128:(k4 + 1) * 128], AF.Identity, [pr, 'MODC', 'MOD1P'], [hres],
                            scale=MOD1P[:, l, kc, si:si + 1], bias=MODC[:, l, kc, si:si + 1])
                bi_, bo_ = blk_of(u)
                kb.dma('sp', HT[bi_][:, :, bo_:bo_ + 128], ho[:], reads=[hres])
            pipeline(len(chunks), ld, cmp_)
        kb.barrier()
        if upto == 'ht':
            return False

        def linear(groups, wsrc_fn, act_src, nk, units):
            with ExitStack() as ph:
                wrot = Rot(nc, ph, 'lw', [128, nk, 512], BF16, 2)
                stg = Rot(nc, ph, 'lstg', [128, 4096], F32, 2)
                arot = Rot(nc, ph, 'la', [128, nk, 512], BF16, 2)
                scr = Rot(nc, ph, 'lscr', [128, 512], F32, 3)
                orot = Rot(nc, ph, 'lo', [128, 512], F32, 3)
                orotb = Rot(nc, ph, 'lob', [128, 512], BF16, 3)
                extra = units(ph) if units else None
                wt = {}

                def ldw(gi):
                    t, r = wrot.nxt(); wt[gi] = (t, r)
                    c = 0
                    for (c0, n) in groups[gi]['cols']:
                        cast_load(t[:, :, c:c + n], wsrc_fn(c0, n), nk, r, stg)
                        c += n

                def cmpg(gi):
                    g = groups[gi]
                    w_t, w_r = wt.pop(gi)
                    at = {}

                    def lda(bi):
                        t, r = arot.nxt(); at[bi] = (t, r)
                        u0, w, si = blocks[bi]
                        kb.dma('sp', t[:, :, 0:w], act_src[blk_of(u0)[0]][:, :, 0:w], writes=[r])

                    def cmpb(bi):
                        a_t, a_r = at.pop(bi)
                        u0, w, si = blocks[bi]
                        g['epi'](dict(w_t=w_t, w_r=w_r, a_t=a_t, a_r=a_r, u0=u0, w=w, si=si, scr=scr, orot=orot,
                                      orotb=orotb, extra=extra, nk=nk))
                    pipeline(len(blocks), lda, cmpb)
                pipeline(len(groups), ldw, cmpg)
            kb.barrier()

        def fmaj_unit(c, ci, nk):
            p, pr = bank()
            for kc in range(nk):
                mm(p[:, 0:c['w']], c['w_t'][:, kc, ci * 128:(ci + 1) * 128], c['a_t'][:, kc, 0:c['w']], kc == 0, kc == nk - 1,
                   [c['w_r'], c['a_r']], [pr])
            return p, pr

        def tmaj_unit(c, ti, ncols, nk):
            p, pr = bank()
            for kc in range(nk):
                mm(p[:, 0:ncols], c['a_t'][:, kc, ti * 128:(ti + 1) * 128], c['w_t'][:, kc, 0:ncols], kc == 0, kc == nk - 1,
                   [c['w_r'], c['a_r']], [pr])
            return p, pr

        def store(dst, src, res):
            kb.dma('sp', dst, src, reads=[res])

        def epi_F(kind, dstT, row0):
            def f(c):
                w = c['w']
                for ci in range(4):
                    p, pr = fmaj_unit(c, ci, c['nk'])
                    rows = slice(row0 + ci * 128, row0 + (ci + 1) * 128)
                    if kind == 'gelu':
                        o, orr = c['orotb'].nxt()
                        gelu_tanh(o[:, 0:w], p[:, 0:w], [pr], [orr], c['scr'], w)
                    elif kind == 'copy':
                        o, orr = c['orotb'].nxt()
                        cp(o[:, 0:w], p[:, 0:w], [pr], [orr])
                    elif kind == 'kscale':
                        o, orr = c['orotb'].nxt()
                        act(o[:, 0:w], p[:, 0:w], AF.Copy, [pr], [orr], scale=float(128 ** -0.5))
                    elif kind == 'sigmoid':
                        o, orr = c['orotb'].nxt()
                        act(o[:, 0:w], p[:, 0:w], AF.Sigmoid, [pr], [orr])
                    store(dstT[rows, c['u0']:c['u0'] + w], o[:, 0:w], orr)
            return f

        def epi_glu(row0):
            def f(c):
                w = c['w']
                for ci in range(2):
                    pa, pra = fmaj_unit(c, ci, c['nk'])
                    pb, prb = fmaj_unit(c, 2 + ci, c['nk'])
                    s, sr = c['scr'].nxt()
                    act(s[:, 0:w], pb[:, 0:w], AF.Sigmoid, [prb], [sr])
                    o, orr = c['orot'].nxt()
                    tt(o[:, 0:w], s[:, 0:w], pa[:, 0:w], ALU.mult, [sr, pra], [orr])
                    store(CG[row0 + ci * 128: row0 + (ci + 1) * 128, c['u0']:c['u0'] + w], o[:, 0:w], orr)
            return f

        def epi_T(kind, ncols, col_dst0):
            def f(c):
                w = c['w']
                for ti in range(w // 128):
                    p, pr = tmaj_unit(c, ti, ncols, c['nk'])
                    rows = slice(c['u0'] + ti * 128, c['u0'] + (ti + 1) * 128)
                    if kind == 'gv':
                        o, orr = c['orot'].nxt()
                        gelu_tanh(o[:, 0:ncols], p[:, 0:ncols], [pr], [orr], c['scr'], ncols)
                        store(GV[rows, col_dst0:col_dst0 + ncols], o[:, 0:ncols], orr)
                    elif kind == 'aq':
                        o, orr = c['orot'].nxt()
                        cp(o[:, 0:ncols], p[:, 0:ncols], [pr], [orr])
                        store(AQ[rows, col_dst0:col_dst0 + ncols], o[:, 0:ncols], orr)
                    elif kind == 'akv':
                        o, orr = c['orot'].nxt()
                        cp(o[:, 0:256], p[:, 0:256], [pr], [orr])
                        store(AK[rows, :], o[:, 0:256], orr)
                        ob, obr = c['orotb'].nxt()
                        act(ob[:, 0:256], p[:, 256:512], AF.Copy, [pr], [obr])
                        store(AV[rows, :], ob[:, 0:256], obr)
                    elif kind == 'mkt':
                        ob, obr = c['orotb'].nxt()
                        act(ob[:, 0:ncols], p[:, 0:ncols], AF.Copy, [pr], [obr], scale=float(128 ** -0.5))
                        store(MKt[rows, col_dst0:col_dst0 + ncols], ob[:, 0:ncols], obr)
                    elif kind == 'mv':
                        ob, obr = c['orotb'].nxt()
                        cp(ob[:, 0:ncols], p[:, 0:ncols], [pr], [obr])
                        store(MV[rows, col_dst0:col_dst0 + ncols], ob[:, 0:ncols], obr)
                    elif kind == 'gt':
                        o, orr = c['orot'].nxt()
                        cp(o[:, 0:32], p[:, 0:32], [pr], [orr])
                        store(GT[rows, :], o[:, 0:32], orr)
            return f

        groups = []
        for i in range(2):
            groups.append(dict(cols=[(OFF['gm_u'] + i * 512, 512)], epi=epi_F('gelu', GU, i * 512)))
            groups.append(dict(cols=[(OFF['ml_q'] + i * 512, 512)], epi=epi_F('copy', MQ, i * 512)))
            groups.append(dict(cols=[(OFF['ml_k'] + i * 512, 512)], epi=epi_F('kscale', MK, i * 512)))
            groups.append(dict(cols=[(OFF['ml_o'] + i * 512, 512)], epi=epi_F('sigmoid', MO, i * 512)))
            groups.append(dict(cols=[(OFF['gm_v'] + i * 512, 512)], epi=epi_T('gv', 512, i * 512)))
            groups.append(dict(cols=[(OFF['att_q'] + i * 512, 512)], epi=epi_T('aq', 512, i * 512)))
            groups.append(dict(cols=[(OFF['ml_k'] + i * 512, 512)], epi=epi_T('mkt', 512, i * 512)))
            groups.append(dict(cols=[(OFF['ml_v'] + i * 512, 512)], epi=epi_T('mv', 512, i * 512)))
        for i in range(4):
            groups.append(dict(cols=[(OFF['cv_a'] + i * 256, 256), (OFF['cv_b'] + i * 256, 256)], epi=epi_glu(i * 256)))
        groups.append(dict(cols=[(OFF['att_k'], 512)], epi=epi_T('akv', 512, 0)))
        groups.append(dict(cols=[(OFF['ml_g'], 32)], epi=epi_T('gt', 32, 0)))
        if proj_sel is not None:
            groups = [groups[v] for v in proj_sel]
        linear(groups, lambda c0, n: w_in[l][:, c0:c0 + n], HT, KC, None)
        if upto == 'proj':
            return False
        return PHASES_REST(l, last, linear, fmaj_unit, tmaj_unit, store, gelu_tanh)

    def PHASES_REST3(l, last, act_chunks, act_blocks):
        def ln_phase(src_fn, g_vec, b_vec, chlist, tag):
            with ExitStack() as ph:
                g_t = ph.enter_context(sbt(nc, tag + 'g', [128, D], F32))
                b_t = ph.enter_context(sbt(nc, tag + 'b', [128, D], F32))
                kb.dma('sp', g_t[:], g_vec.partition_broadcast(128), writes=['lnconst'])
                kb.dma('sp', b_t[:], b_vec.partition_broadcast(128), writes=['lnconst'])
                extra = src_fn(ph, None, None, None, init=True)
                xr = Rot(nc, ph, tag + 'x', [128, D], F32, 2)
                jk = Rot(nc, ph, tag + 'j', [128, D], BF16, 1)
                srot = Rot(nc, ph, tag + 's', [128, 8], F32, 3)
                tl = {}

                def ld(i):
                    u, si = chlist[i]
                    x, xres = xr.nxt(); tl[i] = (x, xres)
                    src_fn(ph, x, xres, (u, si), extra=extra, load=True)

                def cmp_(i):
                    u, si = chlist[i]
                    x, xres = tl.pop(i)
                    src_fn(ph, x, xres, (u, si), extra=extra, load=False)
                    j_, jres = jk.nxt()
                    ln_rows(x[:], D, xres, g_t[:], b_t[:], x[:], xres, srot, j_[:], jres)
                    kb.dma('sp', XA[u:u + 128, :], x[:], reads=[xres])
                pipeline(len(chlist), ld, cmp_)
            kb.barrier()

        def src_r1(ph, x, xres, cu, extra=None, load=True, init=False):
            if init:
                return None
            if load:
                kb.dma('sp', x[:], R1[cu[0]:cu[0] + 128, :], writes=[xres])
        ln_phase(src_r1, ln1_g[l], ln1_b[l], act_chunks, 'l1')
        if upto == 'ln1':
            return False

        for (sn, s0, ntok, si) in streams:
            if last and si == 1:
                continue
            NCs = ntok // 128
            cap = 2 * ntok // NEXP
            JS = min(128, cap); NJ = cap // JS
            with ExitStack() as mo:
                AFF = mo.enter_context(sbt(nc, 'e_aff', [128, NCs, NEXP], F32))
                TA = mo.enter_context(sbt(nc, 'e_ta', [128, NCs, NEXP, 2], F32))
                MASK = mo.enter_context(sbt(nc, 'e_mask', [128, NCs, NEXP], F32))
                SLOT = mo.enter_context(sbt(nc, 'e_slot', [128, NCs, NEXP], F32))
                OFFS = mo.enter_context(sbt(nc, 'e_offs', [128, NCs, NEXP], F32))
                TOT = mo.enter_context(sbt(nc, 'e_tot', [128, NCs, NEXP], F32))
                IDXF = mo.enter_context(sbt(nc, 'e_idxf', [128, NEXP, NJ, 2], F32))
                IDX = mo.enter_context(sbt(nc, 'e_idx', [128, NEXP, NJ], I32))
                IOTA = mo.enter_context(sbt(nc, 'e_iota', [128, 1024], F32))
                TID = mo.enter_context(sbt(nc, 'e_tid', [128, 64], F32))
                kb.dma('sp', IOTA[:], iota_in, writes=['e_iota'])
                kb.dma('sp', TID[:], tid_in, writes=['e_tid'])
                for c_ in range(8):
                    for r0 in range(s0, s0 + ntok, 128):
                        kb.dma('sp', YM[c_][r0:r0 + 128, :], zeros_in[:, 0:512], key='zym')
                with ExitStack() as ph:
                    s2p = ph.enter_context(sbt(nc, 'e_s2p', [128, D], F32))
                    sh2 = ph.enter_context(sbt(nc, 'e_sh2', [128, D], F32))
                    wr_ = ph.enter_context(sbt(nc, 'e_wr', [128, KC, NEXP], F32))
                    kb.dma('sp', s2p[:], MODV[l, si, 4 * D:5 * D].partition_broadcast(128), writes=['e_s2p'])
                    kb.dma('sp', sh2[:], MODV[l, si, 3 * D:4 * D].partition_broadcast(128), writes=['e_sh2'])
                    kb.dma('sp', wr_[:], w_router[l].rearrange('(kc p) e -> p kc e', p=128), writes=['e_wr'])
                    ts(s2p[:], s2p[:], 1.0, None, ALU.add, None, ['e_s2p'], ['e_s2p'], e='pool')
                    xr = Rot(nc, ph, 'e_x', [128, D], F32, 2)
                    xmr = Rot(nc, ph, 'e_xm', [128, D], F32, 1)
                    xbr = Rot(nc, ph, 'e_xb', [128, D], BF16, 2)
                    xtr = Rot(nc, ph, 'e_xt', [128, KC, 128], F32, 2)
                    smr = Rot(nc, ph, 'e_sm', [128, 24], F32, 2)
                    setbanks([0, 1, 2, 3, 4, 5])
                    tl = {}

                    def ld(i):
                        x, xres = xr.nxt(); tl[i] = (x, xres)
                        kb.dma('sp', x[:], XA[s0 + i * 128:s0 + (i + 1) * 128, :], writes=[xres])

                    def cmp_(i):
                        x, xres = tl.pop(i)
                        xm, xmres = xmr.nxt(); xb, xbres = xbr.nxt()
                        tt(xm[:], x[:], s2p[:], ALU.mult, [xres, 'e_s2p'], [xmres])
                        tt(xb[:], xm[:], sh2[:], ALU.add, [xmres, 'e_sh2'], [xbres], e='pool')
                        kb.dma('sp', XM[s0 + i * 128:s0 + (i + 1) * 128, :], xb[:], reads=[xbres])
                        xt, xtres = xtr.nxt()
                        for q in range(8):
                            p, pr = bank()
                            for k4 in range(4):
                                kc = q * 4 + k4
                                tr(p[:, k4 * 128:(k4 + 1) * 128], x[:, kc * 128:(kc + 1) * 128], IDF, [xres, 'CST'], [pr])
                            for k4 in range(4):
                                kc = q * 4 + k4
                                act(xt[:, kc, :], p[:, k4 * 128:(k4 + 1) * 128], AF.Identity, [pr, 'MODC', 'MOD1P'], [xtres],
                                    scale=MOD1P[:, l, 32 + kc, si:si + 1], bias=MODC[:, l, 96 + kc, si:si + 1])
                        pl, plr = PS[6], ('ps', 6)
                        for kc in range(KC):
                            mm(pl[:, 0:NEXP], xt[:, kc, :], wr_[:, kc, :], kc == 0, kc == KC - 1, [xtres, 'e_wr'], [plr])
                        sm, smres = smr.nxt()
                        kb.op('dve', lambda: nc.vector.tensor_reduce(out=sm[:, 0:1], in_=pl[:, 0:NEXP], axis=AX.X, op=ALU.max), [plr], [smres])
                        ts(sm[:, 1:2], sm[:, 0:1], -1.0, None, ALU.mult, None, [smres], [smres])
                        act(sm[:, 8:24], pl[:, 0:NEXP], AF.Exp, [plr, smres], [smres], bias=sm[:, 1:2], accum_out=sm[:, 2:3])
                        kb.op('dve', lambda: nc.vector.reciprocal(out=sm[:, 3:4], in_=sm[:, 2:3]), [smres], [smres])
                        ts(AFF[:, i, :], sm[:, 8:24], sm[:, 3:4], None, ALU.mult, None, [smres], ['e_aff'])
                    pipeline(NCs, ld, cmp_)
                kb.barrier()
                with ExitStack() as ph:
                    lo = ph.enter_context(sbt(nc, 'b_lo', [128, NEXP], F32))
                    hi = ph.enter_context(sbt(nc, 'b_hi', [128, NEXP], F32))
                    mid = ph.enter_context(sbt(nc, 'b_mid', [128, NEXP], F32))
                    ge = ph.enter_context(sbt(nc, 'b_ge', [128, NEXP], F32))
                    d1 = ph.enter_context(sbt(nc, 'b_d1', [128, NEXP], F32))
                    cntp = ph.enter_context(sbt(nc, 'b_cnt', [128, NEXP], F32))
                    cmpt = ph.enter_context(sbt(nc, 'b_cmp', [128, NCs, NEXP], F32))
                    kb.op('pool', lambda: nc.gpsimd.memset(lo[:], 0.0), [], ['b_lo'])
                    kb.op('pool', lambda: nc.gpsimd.memset(hi[:], 1.0), [], ['b_hi'])
                    for it in range(36):
                        tt(mid[:], lo[:], hi[:], ALU.add, ['b_lo', 'b_hi'], ['b_mid'])
                        ts(mid[:], mid[:], 0.5, None, ALU.mult, None, ['b_mid'], ['b_mid'])
                        tt(cmpt[:], AFF[:], mid[:].unsqueeze(1).broadcast_to([128, NCs, NEXP]), ALU.is_ge, ['e_aff', 'b_mid'], ['b_cmp'])
                        kb.op('dve', lambda: nc.vector.tensor_reduce(out=cntp[:], in_=cmpt[:].rearrange('p n e -> p e n'), axis=AX.X, op=ALU.add), ['b_cmp'], ['b_cnt'])
                        pc, pcr = PS[it % 4], ('ps', it % 4)
                        mm(pc[:, 0:NEXP], ONEF, cntp[:], True, True, ['CST', 'b_cnt'], [pcr])
                        ts(ge[:], pc[:, 0:NEXP], float(cap) - 0.5, None, ALU.is_ge, None, [pcr], ['b_ge'])
                        tt(d1[:], mid[:], lo[:], ALU.subtract, ['b_mid', 'b_lo'], ['b_d1'])
                        tt(d1[:], d1[:], ge[:], ALU.mult, ['b_d1', 'b_ge'], ['b_d1'])
                        tt(lo[:], lo[:], d1[:], ALU.add, ['b_lo', 'b_d1'], ['b_lo'])
                        tt(d1[:], hi[:], mid[:], ALU.subtract, ['b_hi', 'b_mid'], ['b_d1'])
                        tt(d1[:], d1[:], ge[:], ALU.mult, ['b_d1', 'b_ge'], ['b_d1'])
                        tt(hi[:], mid[:], d1[:], ALU.add, ['b_mid', 'b_d1'], ['b_hi'])
                    tt(MASK[:], AFF[:], lo[:].unsqueeze(1).broadcast_to([128, NCs, NEXP]), ALU.is_ge, ['e_aff', 'b_lo'], ['e_mask'])
                    NE = NCs * NEXP
                    Mf = MASK[:].rearrange('p n e -> p (n e)')
                    Sf = SLOT[:].rearrange('p n e -> p (n e)')
                    Tf = TOT[:].rearrange('p n e -> p (n e)')
                    for c0 in range(0, NE, 512):
                        c1 = min(NE, c0 + 512)
                        p1, p1r = PS[4], ('ps', 4)
                        p2, p2r = PS[5], ('ps', 5)
                        mm(p1[:, 0:c1 - c0], USTR, Mf[:, c0:c1], True, True, ['CST', 'e_mask'], [p1r])
                        mm(p2[:, 0:c1 - c0], ONEF, Mf[:, c0:c1], True, True, ['CST', 'e_mask'], [p2r])
                        cp(Sf[:, c0:c1], p1[:, 0:c1 - c0], [p1r], ['e_slot'])
                        cp(Tf[:, c0:c1], p2[:, 0:c1 - c0], [p2r], ['e_tot'])
                    kb.op('pool', lambda: nc.gpsimd.memset(OFFS[:, 0, :], 0.0), [], ['e_offs'])
                    for n in range(1, NCs):
                        tt(OFFS[:, n, :], OFFS[:, n - 1, :], TOT[:, n - 1, :], ALU.add, ['e_offs', 'e_tot'], ['e_offs'])
                    tt(SLOT[:], SLOT[:], OFFS[:], ALU.add, ['e_slot', 'e_offs'], ['e_slot'])
                    ts(SLOT[:], SLOT[:], 1.0, None, ALU.add, None, ['e_slot'], ['e_slot'])
                    tt(SLOT[:], SLOT[:], MASK[:], ALU.mult, ['e_slot', 'e_mask'], ['e_slot'])
                    ts(SLOT[:], SLOT[:], -1.0, None, ALU.add, None, ['e_slot'], ['e_slot'])
                    for n in range(NCs):
                        ts(TA[:, n, :, 0], AFF[:, n, :], 0.0, TID[:, n:n + 1], ALU.mult, ALU.add, ['e_aff', 'e_tid'], ['e_ta'], e='pool')
                    ts(TA[:, :, :, 0], TA[:, :, :, 0], float(s0), None, ALU.add, None, ['e_ta'], ['e_ta'], e='pool')
                    cp(TA[:, :, :, 1], AFF[:], ['e_aff'], ['e_ta'], e='pool')
                    selr = Rot(nc, ph, 'e_sel', [128, 128], F32, 3)
                    setbanks([0, 1, 2, 3])
                    for e in range(NEXP):
                        for jc in range(NJ):
                            pi, pir = bank()
                            for n in range(NCs):
                                sl, slres = selr.nxt()
                                ts(sl[:, 0:JS], IOTA[:, jc * JS:(jc + 1) * JS], SLOT[:, n, e:e + 1], None, ALU.is_equal, None, ['e_iota', 'e_slot'], [slres])
                                mm(pi[0:JS, 0:2], sl[:, 0:JS], TA[:, n, e, :], n == 0, n == NCs - 1, [slres, 'e_ta'], [pir])
                            cp(IDXF[0:JS, e, jc, :], pi[0:JS, 0:2], [pir], ['e_idxf'])
                    ts(IDXF[0:JS, :, :, 0], IDXF[0:JS, :, :, 0], 0.25, None, ALU.add, None, ['e_idxf'], ['e_idxf'])
                    cp(IDX[0:JS, :, :], IDXF[0:JS, :, :, 0], ['e_idxf'], ['e_idx'])
                kb.barrier()
                with ExitStack() as ph:
                    xer = Rot(nc, ph, 'e_xe', [128, D], BF16, 1)
                    stg = Rot(nc, ph, 'e_stg', [128, 2048], F32, 2)
                    xeT = ph.enter_context(sbt(nc, 'e_xeT', [128, KC, cap], BF16))
                    wgr = Rot(nc, ph, 'e_wg', [128, KC, 128], BF16, 2)
                    wur = Rot(nc, ph, 'e_wu', [128, KC, 128], BF16, 2)
                    hidT = ph.enter_context(sbt(nc, 'e_hid', [128, 8, cap], BF16))
                    wdr = Rot(nc, ph, 'e_wd', [128, 8, 512], BF16, 2)
                    sgr = Rot(nc, ph, 'e_sg', [128, 512], F32, 2)
                    yer = Rot(nc, ph, 'e_ye', [128, 512], F32, 3)
                    setbanks([0, 1, 2, 3, 4, 5])
                    for e in range(NEXP):
                        for jc in range(NJ):
                            xe, xeres = xer.nxt()
                            kb.dma('pool', xe[0:JS, :], XM[:, :], writes=[xeres], reads=['e_idx'],
                                   indirect=dict(out_offset=None, in_offset=bass.IndirectOffsetOnAxis(ap=IDX[0:JS, e, jc:jc + 1], axis=0)))
                            for k8 in range(4):
                                for kk in range(8):
                                    kc = k8 * 8 + kk
                                    tr(PSB[:, kk * 128:kk * 128 + JS], xe[0:JS, kc * 128:(kc + 1) * 128], IDB[0:JS, 0:JS], [xeres, 'IDB'], ['psb'])
                                cp(xeT[:, k8 * 8:(k8 + 1) * 8, jc * JS:(jc + 1) * JS],
                                   PSB[:, :].rearrange('p (k t) -> p k t', t=128)[:, :, 0:JS], ['psb'], ['e_xeT'])
                        wl = {}

                        def ldw(fc, e=e):
                            wg, wgres = wgr.nxt(); wu, wures = wur.nxt()
                            wl[fc] = (wg, wgres, wu, wures)
                            cast_load(wg[:], w_gate[l][e][:, fc * 128:(fc + 1) * 128], KC, wgres, stg)
                            cast_load(wu[:], w_up[l][e][:, fc * 128:(fc + 1) * 128], KC, wures, stg)

                        def cmpw(fc):
                            wg, wgres, wu, wures = wl.pop(fc)
                            for j0 in range(0, cap, 512):
                                jw = min(512, cap - j0)
                                pg, pgr = bank(); pu, pur = bank()
                                for kc in range(KC):
                                    mm(pg[:, 0:jw], wg[:, kc, :], xeT[:, kc, j0:j0 + jw], kc == 0, kc == KC - 1, [wgres, 'e_xeT'], [pgr])
                                for kc in range(KC):
                                    mm(pu[:, 0:jw], wu[:, kc, :], xeT[:, kc, j0:j0 + jw], kc == 0, kc == KC - 1, [wures, 'e_xeT'], [pur])
                                sg, sgres = sgr.nxt()
                                act(sg[:, 0:jw], pg[:, 0:jw], AF.Silu, [pgr], [sgres])
                                tt(hidT[:, fc, j0:j0 + jw], sg[:, 0:jw], pu[:, 0:jw], ALU.mult, [sgres, pur], ['e_hid'])
                        pipeline(8, ldw, cmpw)
                        dl = {}

                        def ldd(ct, e=e):
                            wd, wdres = wdr.nxt(); dl[ct] = (wd, wdres)
                            cast_load(wd[:], w_down[l][e][:, ct * 512:(ct + 1) * 512], 8, wdres, stg)

                        def cmpd(ct, e=e):
                            wd, wdres = dl.pop(ct)
                            for jc in range(NJ):
                                p, pr = bank()
                                for fc in range(8):
                                    mm(p[0:JS, :], hidT[:, fc, jc * JS:(jc + 1) * JS], wd[:, fc, :], fc == 0, fc == 7, ['e_hid', wdres], [pr])
                                ye, yeres = yer.nxt()
                                ts(ye[0:JS, :], p[0:JS, :], IDXF[0:JS, e, jc, 1:2], None, ALU.mult, None, [pr, 'e_idxf'], [yeres])
                                kb.dma('pool', YM[ct][:, :], ye[0:JS, :], reads=[yeres, 'e_idx'], writes=['YMd'], key=('sc', yeres),
                                       indirect=dict(out_offset=bass.IndirectOffsetOnAxis(ap=IDX[0:JS, e, jc:jc + 1], axis=0), in_offset=None,
                                                     compute_op=ALU.add))
                        pipeline(8, ldd, cmpd)
                kb.barrier()

        def src_x2(ph, x, xres, cu, extra=None, load=True, init=False):
            if init:
                g2 = ph.enter_context(sbt(nc, 'l2_g2', [128, 2, D], F32))
                for s_ in range(2):
                    kb.dma('sp', g2[:, s_, :], MODV[l, s_, 5 * D:6 * D].partition_broadcast(128), writes=['l2_g2'])
                return dict(g2=g2, ym=Rot(nc, ph, 'l2_ym', [128, D], F32, 2), cur={})
            u, si = cu
            if load:
                ym, ymres = extra['ym'].nxt()
                extra['cur'][u] = (ym, ymres)
                kb.dma('sp', x[:], XA[u:u + 128, :], writes=[xres])
                for c_ in range(8):
                    kb.dma('sp', ym[:, c_ * 512:(c_ + 1) * 512], YM[c_][u:u + 128, :], writes=[ymres])
            else:
                ym, ymres = extra['cur'].pop(u)
                tt(ym[:], ym[:], extra['g2'][:, si, :], ALU.mult, [ymres, 'l2_g2'], [ymres])
                stt(x[:], x[:], ALPHA, ym[:], ALU.mult, ALU.add, [xres, ymres], [xres])
        ln_phase(src_x2, ln2_g[l], ln2_b[l], act_chunks, 'l2')
        return True

    def PHASES_REST2(l, last, linear, fmaj_unit, tmaj_unit, store, act_chunks, act_blocks):
        with ExitStack() as ph:
            G = ph.enter_context(sbt(nc, 'm_G', [128, NCH, 32], F32))
            LS = ph.enter_context(sbt(nc, 'm_LS', [128, NCH, 32], F32))
            TMPG = ph.enter_context(sbt(nc, 'm_TG', [128, NCH, 32], F32))
            GB = ph.enter_context(sbt(nc, 'm_GB', [128, 32], F32))
            Bc = ph.enter_context(sbt(nc, 'm_Bc', [128, NCH, 2, 8], F32))
            Bt = ph.enter_context(sbt(nc, 'm_Bt', [128, NCH, 2, 8], F32))
            BI = ph.enter_context(sbt(nc, 'm_BI', [128, NCH, 2, 8], F32))
            WS = ph.enter_context(sbt(nc, 'm_WS', [128, NCH, 2, 8], F32))
            AT = ph.enter_context(sbt(nc, 'm_AT', [128, NCH, 2, 8], F32))
            NG = ph.enter_context(sbt(nc, 'm_NG', [128, 8], F32))
            for n0_ in range(0, NCH, 16):
                n1_ = min(NCH, n0_ + 16)
                kb.dma('sp', G[:, n0_:n1_, :], GT[n0_ * 128:n1_ * 128, :].rearrange('(n p) g -> p n g', p=128), writes=['mG'])
            kb.dma('sp', GB[:], ml_gate_bias[l].partition_broadcast(128), writes=['mGB'])
            kb.dma('sp', NG[:], ml_norm_gc[l], writes=['mNG'])
            tt(G[:], G[:], GB[:].unsqueeze(1).broadcast_to([128, NCH, 32]), ALU.add, ['mG', 'mGB'], ['mG'])
            act(TMPG[:], G[:], AF.Abs, ['mG'], ['mTG'])
            act(TMPG[:], TMPG[:], AF.Exp, ['mTG'], ['mTG'], scale=-1.0)
            act(TMPG[:], TMPG[:], AF.Ln, ['mTG'], ['mTG'], bias=ONEC[:, 0:1])
            ts(LS[:], G[:], 0.0, None, ALU.min, None, ['mG'], ['mLS'])
            tt(LS[:], LS[:], TMPG[:], ALU.subtract, ['mLS', 'mTG'], ['mLS'])
            G4 = G[:].rearrange('p n (a h) -> p n a h', a=4)
            L4 = LS[:].rearrange('p n (a h) -> p n a h', a=4)
            setbanks([0, 1, 2, 3, 4, 5, 6])
            for n in range(NCH):
                p, pr = bank()
                for dr in range(2):
                    U = ULE if dr == 0 else UGE
                    mm(p[:, dr * 8:dr * 8 + 8], U, L4[:, n, 1 + 2 * dr, :], True, True, ['CST', 'mLS'], [pr])
                    mm(p[:, 16 + dr * 8:16 + dr * 8 + 8], ONEF, L4[:, n, 1 + 2 * dr, :], True, True, ['CST', 'mLS'], [pr])
                cp(Bc[:, n, :, :], p[:, 0:16].rearrange('p (a h) -> p a h', a=2), [pr], ['mBc'])
                cp(Bt[:, n, :, :], p[:, 16:32].rearrange('p (a h) -> p a h', a=2), [pr], ['mBt'])
            for dr in range(2):
                tt(BI[:, :, dr, :], G4[:, :, 2 * dr, :], Bc[:, :, dr, :], ALU.subtract, ['mG', 'mBc'], ['mBI'])
            tt(WS[:], Bt[:], BI[:], ALU.add, ['mBt', 'mBI'], ['mWS'])
            act(WS[:], WS[:], AF.Exp, ['mWS'], ['mWS'])
            act(AT[:], Bt[:], AF.Exp, ['mBt'], ['mAT'])

            qTr = Rot(nc, ph, 'm_q', [128, 8, 128], BF16, 2)
            kTr = Rot(nc, ph, 'm_k', [128, 8, 128], BF16, 2)
            ktr = Rot(nc, ph, 'm_kt', [128, BW], BF16, 2)
            vxr = Rot(nc, ph, 'm_vx', [128, 8, 129], BF16, 2)
            hfr = Rot(nc, ph, 'm_hf', [128, 8, 128], F32, 2)
            mor = Rot(nc, ph, 'm_mo', [128, 8, 128], BF16, 2)
            outr = Rot(nc, ph, 'm_out', [128, 8, 128], F32, 2)
            outb = Rot(nc, ph, 'm_outb', [128, 8, 128], BF16, 2)
            lfb = Rot(nc, ph, 'm_lfb', [128, 128], F32, 2)
            ejr = Rot(nc, ph, 'm_ej', [128, 128], F32, 2)
            ar_ = Rot(nc, ph, 'm_a', [128, 128], F32, 2)
            pr_ = Rot(nc, ph, 'm_p', [128, 128], BF16, 2)
            qsr = Rot(nc, ph, 'm_qs', [128, 128], BF16, 2)
            kwr = Rot(nc, ph, 'm_kw', [128, 128], BF16, 2)
            dnr = Rot(nc, ph, 'm_dn', [128, 128], F32, 2)
            hdr = Rot(nc, ph, 'm_hd', [128, 128], F32, 2)
            sqr = Rot(nc, ph, 'm_sq', [128, 128], F32, 2)
            str_ = Rot(nc, ph, 'm_st', [128, 3, 128], F32, 2)
            Cst = ph.enter_context(sbt(nc, 'm_C', [128, 8, 129], F32))
            Cb = ph.enter_context(sbt(nc, 'm_Cb', [128, 8, 128], BF16))
            Nrep = ph.enter_context(sbt(nc, 'm_N', [128, 8, 128], BF16))
            for t_, r_ in zip(vxr.t, range(2)):
                kb.op('pool', lambda: nc.gpsimd.memset(t_[:], 1.0), [], [('m_vx', r_)])
            lat_ch = list(range(0, SEQ // 128)); ctx_ch = list(range(SEQ // 128, NCH))
            for dr in range(2):
                U = ULE if dr == 0 else UGE
                MSK = MLE if dr == 0 else MGE
                kb.op('pool', lambda: nc.gpsimd.memset(Cst[:], 0.0), [], [('mC', h) for h in range(8)])
                kb.op('pool', lambda: nc.gpsimd.memset(Cb[:], 0.0), [], [('mCb', h) for h in range(8)])
                kb.op('pool', lambda: nc.gpsimd.memset(Nrep[:], 0.0), [], [('mCb', h) for h in range(8)])
                order = (ctx_ch + lat_ch) if dr == 0 else (ctx_ch[::-1] + lat_ch[::-1])
                tl = {}

                def ld(i, dr=dr, order=order):
                    n = order[i]; u = n * 128
                    q, qres = qTr.nxt(); k, kres = kTr.nxt(); kt, ktres = ktr.nxt(); vx, vxres = vxr.nxt()
                    ent = [q, qres, k, kres, kt, ktres, vx, vxres]
                    kb.dma('sp', q[:], MQ.rearrange('(h d) t -> d h t', d=128)[:, :, u:u + 128], writes=[qres])
                    kb.dma('sp', k[:], MK.rearrange('(h d) t -> d h t', d=128)[:, :, u:u + 128], writes=[kres])
                    kb.dma('sp', kt[:], MKt[u:u + 128, :], writes=[ktres])
                    kb.dma('sp', vx[:, :, 0:128], MV[u:u + 128, :].rearrange('t (h e) -> t h e', e=128), writes=[vxres])
                    if dr == 1:
                        hf, hfres = hfr.nxt(); mo, mores = mor.nxt()
                        kb.dma('sp', hf[:], HF.rearrange('(h e) t -> e h t', e=128)[:, :, u:u + 128], writes=[hfres])
                        kb.dma('sp', mo[:], MO.rearrange('(h e) t -> e h t', e=128)[:, :, u:u + 128], writes=[mores])
                        ent += [hf, hfres, mo, mores]
                    tl[i] = ent

                def cmp_(i, dr=dr, order=order, U=U, MSK=MSK):
                    n = order[i]; u = n * 128
                    ent = tl.pop(i)
                    q, qres, k, kres, kt, ktres, vx, vxres = ent[:8]
                    if dr == 0:
                        ot, otres = outr.nxt()
                    else:
                        hf, hfres, mo, mores = ent[8:]
                        ob, obres = outb.nxt()
                    for h in range(8):
                        lf = L4[:, n, 1 + 2 * dr, h:h + 1]
                        lb_, lbres = lfb.nxt()
                        act(lb_[:], ONEF, AF.Copy, ['CST', 'mLS'], [lbres], scale=lf)
                        pb, pbr = PS[0], ('ps', 0)
                        mm(pb[:, 0:128], lb_[:], U, True, True, [lbres, 'CST'], [pbr])
                        ej, ejres = ejr.nxt()
                        act(ej[:], pb[:, 0:128], AF.Exp, [pbr], [ejres])
                        a_, ares = ar_.nxt()
                        act(a_[:], pb[:, 0:128], AF.Exp, [pbr, 'mBI'], [ares], bias=BI[:, n, dr, h:h + 1])
                        tt(a_[:], a_[:], MSK, ALU.mult, [ares, 'CST'], [ares], e='pool')
                        pst, pstr = PS[1], ('ps', 1)
                        mm(pst[:, 0:128], k[:, h, :], q[:, h, :], True, True, [kres, qres], [pstr])
                        p_, pres = pr_.nxt()
                        tt(p_[:], pst[:, 0:128], a_[:], ALU.mult, [pstr, ares], [pres])
                        qs, qsres = qsr.nxt()
                        tt(qs[:], q[:, h, :], ej[:], ALU.mult, [qres, ejres], [qsres], e='pool')
                        pn, pnr = PS[2], ('ps', 2)
                        mm(pn[:, 0:128], vx[:, h, 0:128], p_[:], True, False, [vxres, pres], [pnr])
                        mm(pn[:, 0:128], Cb[:, h, :], qs[:], False, True, [('mCb', h), qsres], [pnr])
                        pd, pdr = PS[3], ('ps', 3)
                        mm(pd[:, 0:128], ONEB[:], p_[:], True, False, ['ONEB', pres], [pdr])
                        mm(pd[:, 0:128], Nrep[:, h, :], qs[:], False, True, [('mCb', h), qsres], [pdr])
                        dn, dnres = dnr.nxt()
                        act(dn[:], pd[:, 0:128], AF.Abs, [pdr], [dnres])
                        ts(dn[:], dn[:], 1.0, None, ALU.max, None, [dnres], [dnres])
                        kb.op('dve', lambda: nc.vector.reciprocal(out=dn[:], in_=dn[:]), [dnres], [dnres])
                        if dr == 0:
                            tt(ot[:, h, :], pn[:, 0:128], dn[:], ALU.mult, [pnr, dnres], [otres])
                        else:
                            hd, hdres = hdr.nxt()
                            tt(hd[:], pn[:, 0:128], dn[:], ALU.mult, [pnr, dnres], [hdres])
                            tt(hd[:], hd[:], hf[:, h, :], ALU.add, [hdres, hfres], [hdres])
                            sq, sqres = sqr.nxt()
                            act(sq[:], hd[:], AF.Square, [hdres], [sqres])
                            pm, pmr = PS[5], ('ps', 5)
                            pq, pqr = PS[6], ('ps', 6)
                            mm(pm[:, 0:128], ONEF, hd[:], True, True, ['CST', hdres], [pmr])
                            mm(pq[:, 0:128], ONEF, sq[:], True, True, ['CST', sqres], [pqr])
                            s_, sres = str_.nxt()
                            ts(s_[:, 0, :], pm[:, 0:128], 1.0 / 128, None, ALU.mult, None, [pmr], [sres])
                            tt(s_[:, 1, :], s_[:, 0, :], s_[:, 0, :], ALU.mult, [sres], [sres])
                            stt(s_[:, 2, :], pq[:, 0:128], 1.0 / 128, s_[:, 1, :], ALU.mult, ALU.subtract, [pqr, sres], [sres])
                            act(s_[:, 2, :], s_[:, 2, :], AF.Sqrt, [sres], [sres], bias=EPSC[:, 0:1])
                            kb.op('dve', lambda: nc.vector.reciprocal(out=s_[:, 1, :], in_=s_[:, 2, :]), [sres], [sres])
                            tt(hd[:], hd[:], s_[:, 0, :], ALU.subtract, [hdres, sres], [hdres])
                            tt(hd[:], hd[:], s_[:, 1, :], ALU.mult, [hdres, sres], [hdres])
                            stt(ob[:, h, :], hd[:], NG[:, h:h + 1], mo[:, h, :], ALU.mult, ALU.mult, [hdres, 'mNG', mores], [obres])
                        kw, kwres = kwr.nxt()
                        act(kw[:], kt[:, h * 128:(h + 1) * 128], AF.Copy, [ktres, 'mWS'], [kwres], scale=WS[:, n, dr, h:h + 1])
                        pc, pcr = PS[4], ('ps', 4)
                        mm(pc[:, 0:129], kw[:], vx[:, h, :], True, True, [kwres, vxres], [pcr])
                        stt(Cst[:, h, :], Cst[:, h, :], AT[:, n, dr, h:h + 1], pc[:, 0:129], ALU.mult, ALU.add, [('mC', h), 'mAT', pcr], [('mC', h)])
                        cp(Cb[:, h, :], Cst[:, h, 0:128], [('mC', h)], [('mCb', h)], e='pool')
                        act(Nrep[:, h, :], ONEF, AF.Copy, ['CST', ('mC', h)], [('mCb', h)], scale=Cst[:, h, 128:129])
                    if dr == 0:
                        kb.dma('sp', HF.rearrange('(h e) t -> e h t', e=128)[:, :, u:u + 128], ot[:], reads=[otres])
                    else:
                        kb.dma('sp', BR[3].rearrange('(h e) t -> e h t', e=128)[:, :, u:u + 128], ob[:], reads=[obres])
                pipeline(len(order), ld, cmp_)
                kb.barrier()
        kb.barrier()
        if upto == 'mlstm':
            return False

        with ExitStack() as ph:
            wgr = Rot(nc, ph, 'mg_wg', [128, KC, 4, 128], BF16, 1)
            wbr = Rot(nc, ph, 'mg_wb', [128, 8, 4, 128], BF16, 1)
            hr = Rot(nc, ph, 'mg_h', [128, KC, 512], BF16, 2)
            brr = Rot(nc, ph, 'mg_b', [128, 4, 8, 512], BF16, 2)
            sgr = Rot(nc, ph, 'mg_sg', [128, 512], F32, 2)
            stg = Rot(nc, ph, 'mg_stg', [128, 2048], F32, 2)
            accr = Rot(nc, ph, 'mg_acc', [128, 512], F32, 2)
            yor = Rot(nc, ph, 'mg_yo', [128, 512], BF16, 2)
            setbanks([0, 1, 2, 3, 4, 5])
            wl = {}

            def ldw(nch):
                wg, wgres = wgr.nxt(); wb, wbres = wbr.nxt()
                wl[nch] = (wg, wgres, wb, wbres)
                for i in range(4):
                    c0 = OFF['merge'] + i * D + nch * 128
                    cast_load(wg[:, :, i, :], w_in[l][:, c0:c0 + 128], KC, wgres, stg)
                    cast_load(wb[:, :, i, :], w_branch[l][i][:, nch * 128:(nch + 1) * 128], 8, wbres, stg)

            def cmpw(nch):
                wg, wgres, wb, wbres = wl.pop(nch)
                al = {}

                def lda(bi):
                    u0, w, si = act_blocks[bi]
                    h_, hres = hr.nxt(); b_, bres = brr.nxt()
                    al[bi] = (h_, hres, b_, bres)
                    kb.dma('sp', h_[:, :, 0:w], HT[blk_of(u0)[0]][:, :, 0:w], writes=[hres])
                    for i in range(4):
                        kb.dma('sp', b_[:, i, :, 0:w], BR[i].rearrange('(kc p) t -> p kc t', p=128)[:, :, u0:u0 + w], writes=[bres])

                def cmpa(bi):
                    u0, w, si = act_blocks[bi]
                    h_, hres, b_, bres = al.pop(bi)
                    acc, accres = accr.nxt()
                    for i in range(4):
                        pg, pgr = bank()
                        for kc in range(KC):
                            mm(pg[:, 0:w], wg[:, kc, i, :], h_[:, kc, 0:w], kc == 0, kc == KC - 1, [wgres, hres], [pgr])
                        pb, pbr = bank()
                        for kc in range(8):
                            mm(pb[:, 0:w], wb[:, kc, i, :], b_[:, i, kc, 0:w], kc == 0, kc == 7, [wbres, bres], [pbr])
                        sg, sgres = sgr.nxt()
                        act(sg[:, 0:w], pg[:, 0:w], AF.Sigmoid, [pgr], [sgres])
                        if i == 0:
                            tt(acc[:, 0:w], sg[:, 0:w], pb[:, 0:w], ALU.mult, [sgres, pbr], [accres])
                        else:
                            tt(sg[:, 0:w], sg[:, 0:w], pb[:, 0:w], ALU.mult, [sgres, pbr], [sgres])
                            tt(acc[:, 0:w], acc[:, 0:w], sg[:, 0:w], ALU.add, [accres, sgres], [accres], e='pool')
                    yo, yores = yor.nxt()
                    cp(yo[:, 0:w], acc[:, 0:w], [accres], [yores], e='pool')
                    kb.dma('sp', YT[blk_of(u0)[0]][:, nch, 0:w], yo[:, 0:w], reads=[yores])
                pipeline(len(act_blocks), lda, cmpa)
            pipeline(KC, ldw, cmpw, depth=0)
        kb.barrier()
        if upto == 'merge':
            return False

        def units_wout(ph):
            return dict(gt=Rot(nc, ph, 'wo_g', [128, 512], F32, 2), xa=Rot(nc, ph, 'wo_x', [128, 512], F32, 3))

        def epi_wout(ct):
            def f(c):
                w = c['w']; si = c['si']
                if last and si == 1:
                    return
                gt, gtres = c['extra']['gt'].nxt()
                kb.dma('sp', gt[:], MODV[l, si, 2 * D + ct * 512: 2 * D + (ct + 1) * 512].partition_broadcast(128), writes=[gtres])
                for ti in range(w // 128):
                    rows = slice(c['u0'] + ti * 128, c['u0'] + (ti + 1) * 128)
                    xa, xares = c['extra']['xa'].nxt()
                    kb.dma('sp', xa[:], XA[rows, ct * 512:(ct + 1) * 512], writes=[xares])
                    p, pr = tmaj_unit(c, ti, 512, c['nk'])
                    o, orr = c['orot'].nxt()
                    tt(o[:], p[:, :], gt[:], ALU.mult, [pr, gtres], [orr])
                    stt(o[:], xa[:], ALPHA, o[:], ALU.mult, ALU.add, [xares, orr], [orr])
                    store(R1[rows, ct * 512:(ct + 1) * 512], o[:], orr)
            return f
        setbanks([0, 1, 2, 3, 4, 5])
        linear([dict(cols=[(ct * 512, 512)], epi=epi_wout(ct)) for ct in range(8)], lambda c0, n: w_out[l][:, c0:c0 + n], YT, KC, units_wout)
        if upto == 'wout':
            return False
        return PHASES_REST3(l, last, act_chunks, act_blocks)

    def setbanks(lst):
        bankset[0] = list(lst)
        psi[0] = 0

    def ln_rows(x, n, xres, g_t, b_t, out, outres, strot, junk, junkres):
        s, sr = strot.nxt()
        kb.op('dve', lambda: nc.vector.tensor_reduce(out=s[:, 0:1], in_=x, axis=AX.X, op=ALU.add), [xres], [sr])
        ts(s[:, 1:2], s[:, 0:1], -1.0 / n, None, ALU.mult, None, [sr], [sr])
        act(x, x, AF.Identity, [xres, sr], [xres], bias=s[:, 1:2])
        act(junk, x, AF.Square, [xres], [junkres, sr], accum_out=s[:, 2:3])
        act(s[:, 3:4], s[:, 2:3], AF.Sqrt, [sr], [sr], scale=1.0 / n, bias=EPSC[:, 0:1])
        kb.op('dve', lambda: nc.vector.reciprocal(out=s[:, 4:5], in_=s[:, 3:4]), [sr], [sr])
        stt(out, x, s[:, 4:5], g_t, ALU.mult, ALU.mult, [xres, sr, 'lnconst'], [outres])
        tt(out, out, b_t, ALU.add, [outres, 'lnconst'], [outres])

    def PHASES_REST(l, last, linear, fmaj_unit, tmaj_unit, store, gelu_tanh):
        act_chunks = [(u, si) for (u, si) in chunks if (si == 0 or not last)]
        act_blocks = [b for b in blocks if (b[2] == 0 or not last)]
        with ExitStack() as ph:
            setbanks([0, 1, 2, 3, 4, 5])
            lg = ph.enter_context(sbt(nc, 'g_lg', [128, BW], F32))
            lb = ph.enter_context(sbt(nc, 'g_lb', [128, BW], F32))
            bsb = ph.enter_context(sbt(nc, 'g_bs', [128, BW], F32))
            wst = ph.enter_context(sbt(nc, 'g_ws', [128, 8, 128], BF16))
            kb.dma('sp', lg[:], gm_ln_g[l].partition_broadcast(128), writes=['lnconst'])
            kb.dma('sp', lb[:], gm_ln_b[l].partition_broadcast(128), writes=['lnconst'])
            kb.dma('sp', bsb[:], gm_b_s[l].partition_broadcast(128), writes=['lnconst'])
            for g_ in range(8):
                kb.dma('pool', wst[:, g_, :], gm_wsT[l][g_], writes=['g_ws'])
            vr = Rot(nc, ph, 'gv', [128, BW], F32, 2)
            vn = Rot(nc, ph, 'gvn', [128, BW], BF16, 2)
            jk = Rot(nc, ph, 'gjk', [128, BW], BF16, 1)
            ur = Rot(nc, ph, 'gu', [128, 8, 128], BF16, 2)
            orr_ = Rot(nc, ph, 'go', [128, 8, 128], BF16, 2)
            tmp = Rot(nc, ph, 'gtmp', [128, BW], F32, 2)
            srot = Rot(nc, ph, 'gst', [128, 8], F32, 3)
            tl = {}

            def ld(i):
                u, si = act_chunks[i]
                v, vres = vr.nxt(); ut, ures = ur.nxt()
                tl[i] = (v, vres, ut, ures)
                kb.dma('sp', v[:], GV[u:u + 128, :], writes=[vres])
                kb.dma('sp', ut[:], GU.rearrange('(g c) t -> c g t', c=128)[:, :, u:u + 128], writes=[ures])

            def cmp_(i):
                u, si = act_chunks[i]
                v, vres, ut, ures = tl.pop(i)
                n_, nres = vn.nxt()
                j_, jres = jk.nxt()
                ln_rows(v[:], BW, vres, lg[:], lb[:], v[:], vres, srot, j_[:], jres)
                cp(n_[:], v[:], [vres], [nres], e='pool')
                o, ores = orr_.nxt()
                t_, tres = tmp.nxt()
                for hf in range(2):
                    p, pr = bank()
                    for g4 in range(4):
                        g = hf * 4 + g4
                        mm(p[:, g4 * 128:(g4 + 1) * 128], n_[:, g * 128:(g + 1) * 128], wst[:, g, :], True, True, [nres, 'g_ws'], [pr])
                    sl = slice(hf * 512, (hf + 1) * 512)
                    tt(t_[:, sl], p[:, :], bsb[:, sl], ALU.add, [pr, 'lnconst'], [tres])
                    tt(o[:].rearrange('c g p -> c (g p)')[:, sl], t_[:, sl], ut[:].rearrange('c g p -> c (g p)')[:, sl], ALU.mult, [tres, ures], [ores])
                kb.dma('sp', BR[0].rearrange('(g c) t -> c g t', c=128)[:, :, u:u + 128], o[:], reads=[ores])
            pipeline(len(act_chunks), ld, cmp_)
        kb.barrier()
        if upto == 'gmlp':
            return False

        with ExitStack() as ph:
            gq = ph.enter_context(sbt(nc, 'a_gq', [128, 128], F32))
            gk = ph.enter_context(sbt(nc, 'a_gk', [128, 128], F32))
            kb.dma('sp', gq[:], aq_gain[l].partition_broadcast(128), writes=['again'])
            kb.dma('sp', gk[:], ak_gain[l].partition_broadcast(128), writes=['again'])
            qr = Rot(nc, ph, 'aq', [128, 10, 128], F32, 2)
            cr = Rot(nc, ph, 'acs', [128, 2, 64], F32, 2)
            sqr = Rot(nc, ph, 'asq', [128, 10, 128], F32, 1)
            qn = Rot(nc, ph, 'aqn', [128, 10, 128], F32, 1)
            t1r = Rot(nc, ph, 'at1', [128, 10, 64], F32, 2)
            qf = Rot(nc, ph, 'aqf', [128, 10, 128], BF16, 2)
            qT = Rot(nc, ph, 'aqT', [128, 10, 128], BF16, 2)
            srot = Rot(nc, ph, 'ast', [128, 32], F32, 2)
            tl = {}

            def ld(i):
                u, si = chunks[i]
                q, qres = qr.nxt(); c_, cres = cr.nxt()
                tl[i] = (q, qres, c_, cres)
                kb.dma('sp', q[:, 0:8, :], AQ[u:u + 128, :].rearrange('t (h d) -> t h d', d=128), writes=[qres])
                kb.dma('sp', q[:, 8:10, :], AK[u:u + 128, :].rearrange('t (h d) -> t h d', d=128), writes=[qres])
                if si == 0:
                    kb.dma('sp', c_[:, 0, :], ropeC[u:u + 128, :], writes=[cres])
                    kb.dma('sp', c_[:, 1, :], ropeS[u:u + 128, :], writes=[cres])

            def cmp_(i):
                u, si = chunks[i]
                q, qres, c_, cres = tl.pop(i)
                sq, sqres = sqr.nxt()
                s, sres = srot.nxt()
                tt(sq[:], q[:], q[:], ALU.mult, [qres], [sqres])
                kb.op('dve', lambda: nc.vector.tensor_reduce(out=s[:, 0:10], in_=sq[:], axis=AX.X, op=ALU.add), [sqres], [sres])
                act(s[:, 10:20], s[:, 0:10], AF.Sqrt, [sres], [sres], scale=1.0 / 128, bias=EPSC[:, 0:1])
                kb.op('dve', lambda: nc.vector.reciprocal(out=s[:, 20:30], in_=s[:, 10:20]), [sres], [sres])
                n_, nres = qn.nxt()
                tt(n_[:], q[:], s[:, 20:30].unsqueeze(2).broadcast_to([128, 10, 128]), ALU.mult, [qres, sres], [nres])
                tt(n_[:, 0:8, :], n_[:, 0:8, :], gq[:].unsqueeze(1).broadcast_to([128, 8, 128]), ALU.mult, [nres, 'again'], [nres])
                tt(n_[:, 8:10, :], n_[:, 8:10, :], gk[:].unsqueeze(1).broadcast_to([128, 2, 128]), ALU.mult, [nres, 'again'], [nres])
                f_, fres = qf.nxt()
                if si == 0:
                    nv = n_[:].rearrange('p h (a b d) -> p h a b d', a=2, b=2)
                    fv = f_[:].rearrange('p h (a b d) -> p h a b d', a=2, b=2)
                    x1 = nv[:, :, :, 0, :]; x2 = nv[:, :, :, 1, :]
                    C = c_[:, 0, :].rearrange('p (a d) -> p a d', a=2).unsqueeze(1).broadcast_to([128, 10, 2, 32])
                    S = c_[:, 1, :].rearrange('p (a d) -> p a d', a=2).unsqueeze(1).broadcast_to([128, 10, 2, 32])
                    ta, tar = t1r.nxt(); tb, tbr = t1r.nxt()
                    tav = ta[:].rearrange('p h (a d) -> p h a d', a=2); tbv = tb[:].rearrange('p h (a d) -> p h a d', a=2)
                    tt(tav, x1, C, ALU.mult, [nres, cres], [tar])
                    tt(tbv, x2, S, ALU.mult, [nres, cres], [tbr])
                    tt(fv[:, :, :, 0, :], tav, tbv, ALU.subtract, [tar, tbr], [fres])
                    tt(tav, x2, C, ALU.mult, [nres, cres], [tar])
                    tt(tbv, x1, S, ALU.mult, [nres, cres], [tbr])
                    tt(fv[:, :, :, 1, :], tav, tbv, ALU.add, [tar, tbr], [fres])
                else:
                    cp(f_[:], n_[:], [nres], [fres])
                o, ores = qT.nxt()
                for hh in range(10):
                    b0 = (hh % 8) * 128
                    tr(PSB[:, b0:b0 + 128], f_[:, hh, :], IDB[:], [fres, 'IDB'], ['psb'])
                    if hh == 7:
                        cp(o[:, 0:8, :], PSB[:, 0:1024].rearrange('p (h t) -> p h t', t=128), ['psb'], [ores])
                cp(o[:, 8:10, :], PSB[:, 0:256].rearrange('p (h t) -> p h t', t=128), ['psb'], [ores])
                kb.dma('sp', QT.rearrange('(h d) t -> d h t', d=128)[:, :, u:u + 128], o[:, 0:8, :], reads=[ores])
                kb.dma('sp', KT.rearrange('(h d) t -> d h t', d=128)[:, :, u:u + 128], o[:, 8:10, :], reads=[ores])
            pipeline(len(chunks), ld, cmp_)
        kb.barrier()

        with ExitStack() as ph:
            kts = ph.enter_context(sbt(nc, 'at_k', [128, 2, T], BF16))
            vs = ph.enter_context(sbt(nc, 'at_v', [128, NCH, 256], BF16))
            kb.dma('sp', kts[:], KT.rearrange('(g d) t -> d g t', d=128), writes=['at_k'])
            for n0_ in range(0, NCH, 8):
                n1_ = min(NCH, n0_ + 8)
                kb.dma('sp', vs[:, n0_:n1_, :], AV[n0_ * 128:n1_ * 128, :].rearrange('(n p) c -> p n c', p=128), writes=['at_v'])
            qb = Rot(nc, ph, 'at_q', [128, 512], BF16, 3)
            ptr_ = Rot(nc, ph, 'at_p', [128, 512], BF16, 3)
            rcr = Rot(nc, ph, 'at_rc', [128, 512], F32, 2)
            obr = Rot(nc, ph, 'at_o', [128, 512], BF16, 2)
            setbanks([0, 1, 2])
            work = []
            for (u0, w, si) in act_blocks:
                keych = list(range(NCH)) if si == 0 else list(range(SEQ // 128, NCH))
                for h in range(8):
                    work.append((u0, w, si, h, keych))
            tl = {}

            def ld(i):
                u0, w, si, h, keych = work[i]
                q, qres = qb.nxt(); tl[i] = (q, qres)
                kb.dma('sp', q[:, 0:w], QT[h * 128:(h + 1) * 128, u0:u0 + w], writes=[qres])

            def cmp_(i):
                u0, w, si, h, keych = work[i]
                q, qres = tl.pop(i)
                g = h // 4
                PO, POr = PS[3 + (i % 2)], ('ps', 3 + (i % 2))
                PM, PMr = PS[5 + (i % 2)], ('ps', 5 + (i % 2))
                for ki, n in enumerate(keych):
                    p, pr = bank()
                    mm(p[:, 0:w], kts[:, g, n * 128:(n + 1) * 128], q[:, 0:w], True, True, ['at_k', qres], [pr])
                    pt, ptres = ptr_.nxt()
                    act(pt[:, 0:w], p[:, 0:w], AF.Exp, [pr], [ptres], scale=float(128 ** -0.5))
                    mm(PO[:, 0:w], vs[:, n, g * 128:(g + 1) * 128], pt[:, 0:w], ki == 0, ki == len(keych) - 1, ['at_v', ptres], [POr])
                    mm(PM[:, 0:w], ONEB[:], pt[:, 0:w], ki == 0, ki == len(keych) - 1, ['ONEB', ptres], [PMr])
                rc, rcres = rcr.nxt()
                kb.op('dve', lambda: nc.vector.reciprocal(out=rc[:, 0:w], in_=PM[:, 0:w]), [PMr], [rcres])
                o, ores = obr.nxt()
                tt(o[:, 0:w], PO[:, 0:w], rc[:, 0:w], ALU.mult, [POr, rcres], [ores])
                kb.dma('sp', BR[1][h * 128:(h + 1) * 128, u0:u0 + w], o[:, 0:w], reads=[ores])
            pipeline(len(work), ld, cmp_)
        kb.barrier()
        if upto == 'att':
            return False

        with ExitStack() as ph:
            wT = ph.enter_context(sbt(nc, 'c_w', [128, 8, 31], F32))
            cb = ph.enter_context(sbt(nc, 'c_b', [128, 8], F32))
            cg_ = ph.enter_context(sbt(nc, 'c_g', [128, 8], F32))
            cbb = ph.enter_context(sbt(nc, 'c_bb', [128, 8], F32))
            kb.dma('sp', wT[:], conv_wT[l].rearrange('(cc c) j -> c cc j', c=128), writes=['cconst'])
            kb.dma('sp', cb[:], conv_bc[l], writes=['cconst'])
            kb.dma('sp', cg_[:], conv_ln_gc[l], writes=['cconst'])
            kb.dma('sp', cbb[:], conv_ln_bc[l], writes=['cconst'])
            gin = Rot(nc, ph, 'c_in', [128, 8, 542], F32, 2)
            Y = ph.enter_context(sbt(nc, 'c_y', [128, 8, 512], F32))
            Y2 = ph.enter_context(sbt(nc, 'c_y2', [128, 8, 512], F32))
            mt = Rot(nc, ph, 'c_m', [128, 4, 512], F32, 1)
            zr = Rot(nc, ph, 'c_z', [128, 512], F32, 2)
            zo = Rot(nc, ph, 'c_zo', [128, 512], BF16, 2)
            srng = {0: (0, SEQ), 1: (SEQ, T)}
            tl = {}

            def ld(i):
                u0, w, si = act_blocks[i]
                g_, gres = gin.nxt(); tl[i] = (g_, gres)
                s0, s1 = srng[si]
                a = max(s0, u0 - 15); b = min(s1, u0 + w + 15)
                kb.op('pool', lambda: nc.gpsimd.memset(g_[:], 0.0), [], [gres])
                off = a - (u0 - 15)
                kb.dma('sp', g_[:, :, off:off + (b - a)], CG.rearrange('(cc c) t -> c cc t', c=128)[:, :, a:b], writes=[gres])

            def cmp_(i):
                u0, w, si = act_blocks[i]
                g_, gres = tl.pop(i)
                for cc in range(8):
                    ts(Y[:, cc, 0:w], g_[:, cc, 0:w], wT[:, cc, 0:1], cb[:, cc:cc + 1], ALU.mult, ALU.add, [gres, 'cconst'], [('cy', cc)])
                    for j in range(1, 31):
                        stt(Y[:, cc, 0:w], g_[:, cc, j:j + w], wT[:, cc, j:j + 1], Y[:, cc, 0:w], ALU.mult, ALU.add, [gres, 'cconst', ('cy', cc)], [('cy', cc)])
                    act(Y2[:, cc, 0:w], Y[:, cc, 0:w], AF.Square, [('cy', cc)], [('cy2', cc)])
                S1, S1r = PS[0], ('ps', 0)
                S2, S2r = PS[1], ('ps', 1)
                for cc in range(8):
                    mm(S1[:, 0:w], ONEF, Y[:, cc, 0:w], cc == 0, cc == 7, ['CST', ('cy', cc)], [S1r])
                for cc in range(8):
                    mm(S2[:, 0:w], ONEF, Y2[:, cc, 0:w], cc == 0, cc == 7, ['CST', ('cy2', cc)], [S2r])
                m_, mres = mt.nxt()
                ts(m_[:, 0, 0:w], S1[:, 0:w], 1.0 / BW, None, ALU.mult, None, [S1r], [mres])
                tt(m_[:, 1, 0:w], m_[:, 0, 0:w], m_[:, 0, 0:w], ALU.mult, [mres], [mres])
                stt(m_[:, 2, 0:w], S2[:, 0:w], 1.0 / BW, m_[:, 1, 0:w], ALU.mult, ALU.subtract, [S2r, mres], [mres])
                act(m_[:, 2, 0:w], m_[:, 2, 0:w], AF.Sqrt, [mres], [mres], bias=EPSC[:, 0:1])
                kb.op('dve', lambda: nc.vector.reciprocal(out=m_[:, 3, 0:w], in_=m_[:, 2, 0:w]), [mres], [mres])
                for cc in range(8):
                    z, zres = zr.nxt()
                    tt(z[:, 0:w], Y[:, cc, 0:w], m_[:, 0, 0:w], ALU.subtract, [('cy', cc), mres], [zres])
                    tt(z[:, 0:w], z[:, 0:w], m_[:, 3, 0:w], ALU.mult, [zres, mres], [zres])
                    o, ores = zo.nxt()
                    act(z[:, 0:w], z[:, 0:w], AF.Identity, [zres, 'cconst'], [zres], scale=cg_[:, cc:cc + 1], bias=cbb[:, cc:cc + 1])
                    act(o[:, 0:w], z[:, 0:w], AF.Silu, [zres], [ores])
                    kb.dma('sp', BR[2][cc * 128:(cc + 1) * 128, u0:u0 + w], o[:, 0:w], reads=[ores])
            pipeline(len(act_blocks), ld, cmp_)
        kb.barrier()
        if upto == 'conv':
            return False
        return PHASES_REST2(l, last, linear, fmaj_unit, tmaj_unit, store, act_chunks, act_blocks)


    ok = True
    for l in range(L):
        if not layer(l):
            ok = False
            break
    if ok:
        for r0_ in range(0, SEQ, 512):
            kb.dma('sp', out_d[r0_:r0_ + 512, :], XA[r0_:r0_ + 512, :], key='fin')
    kb.barrier()
    st0.close()
    build.ninst = kb.ninst
    return nc


def prep_inputs(inp, SEQ, CTX, L):
    f = np.float32
    d = {}
    d['x'] = np.ascontiguousarray(inp['x'].reshape(SEQ, D), f)
    d['ctx'] = np.ascontiguousarray(inp['ctx'].reshape(CTX, D), f)
    cc = np.stack([inp['c'].reshape(D), inp['c_ctx'].reshape(D)], axis=-1)
    d['cc'] = np.ascontiguousarray(cc.reshape(KC, 128, 2).transpose(1, 0, 2), f)
    d['w_mod'] = inp['w_mod'][:L]
    d['b_modc'] = np.ascontiguousarray(inp['b_mod'][:L].reshape(L, 192, 128).transpose(0, 2, 1), f)
    d['w_in'] = inp['w_in'][:L]
    for k in ['att_q_gain', 'att_k_gain', 'gm_ln_g', 'gm_ln_b', 'w_branch', 'w_out', 'ln1_g', 'ln1_b',
              'w_router', 'w_gate', 'w_up', 'w_down', 'ln2_g', 'ln2_b']:
        d[k] = inp[k][:L]
    d['gm_wsT'] = np.ascontiguousarray(inp['gm_w_s'][:L].transpose(0, 1, 3, 2), f)
    d['gm_b_s'] = np.ascontiguousarray(inp['gm_b_s'][:L].reshape(L, 1024), f)
    d['conv_wT'] = np.ascontiguousarray(inp['conv_w'][:L].transpose(0, 2, 1), f)
    for k, kk in [('conv_b', 'conv_bc'), ('conv_ln_g', 'conv_ln_gc'), ('conv_ln_b', 'conv_ln_bc'), ('ml_norm_g', 'ml_norm_gc')]:
        d[kk] = np.ascontiguousarray(inp[k][:L].reshape(L, 8, 128).transpose(0, 2, 1), f)
    d['ml_gate_bias'] = np.ascontiguousarray(inp['ml_gate_bias'][:L].reshape(L, 32), f)
    cst = np.zeros((128, 8, 128), f)
    i = np.arange(128)
    cst[:, 0] = np.eye(128)
    cst[:, 1] = (i[:, None] <= i[None, :])
    cst[:, 2] = (i[:, None] >= i[None, :])
    cst[:, 3] = 1.0
    cst[:, 4] = (i[:, None] <= i[None, :])
    cst[:, 5] = (i[:, None] >= i[None, :])
    cst[:, 6] = (i[:, None] < i[None, :])
    d['cst'] = cst
    t = np.arange(SEQ)
    row = (t // 64).astype(np.float64); col = (t % 64).astype(np.float64)
    inv = 10000.0 ** (-np.arange(32, dtype=np.float64) / 32)
    ar = (row[:, None].astype(f) * inv[None, :].astype(f)).astype(f)
    ac = (col[:, None].astype(f) * inv[None, :].astype(f)).astype(f)
    d['ropeC'] = np.concatenate([np.cos(ar), np.cos(ac)], axis=1).astype(f)
    d['ropeS'] = np.concatenate([np.sin(ar), np.sin(ac)], axis=1).astype(f)
    d['iota'] = np.ascontiguousarray(np.broadcast_to(np.arange(1024, dtype=f)[None, :], (128, 1024)))
    d['tid'] = (np.arange(64)[None, :] * 128 + np.arange(128)[:, None]).astype(f)
    d['zeros'] = np.zeros((128, 4096), f)
    return d


_CACHE = {}


def kernel(**inputs):
    SEQ, CTX, L = 8192, 256, 2
    if 'nc' not in _CACHE:
        _CACHE['nc'] = build(SEQ, CTX, L)
    d = prep_inputs(inputs, SEQ, CTX, L)
    res = run_bass_kernel_spmd(_CACHE['nc'], [d], core_ids=[0])
    return np.asarray(res.results[0]['out'], np.float32).reshape(1, SEQ, D)
```

```python
import numpy as np
from contextlib import ExitStack
import concourse.bass as bass
import concourse.mybir as mybir
from concourse.bass_utils import run_bass_kernel_spmd

F32 = mybir.dt.float32
BF16 = mybir.dt.bfloat16
I32 = mybir.dt.int32
AF = mybir.ActivationFunctionType
ALU = mybir.AluOpType
AX = mybir.AxisListType

D = 4096
KC = 32
BW = 1024
NEXP = 16
FF = 1024
EPS = 1e-6
OFF = dict(gm_u=0, gm_v=1024, att_q=2048, att_k=3072, att_v=3328, cv_a=3584, cv_b=4608,
           ml_q=5632, ml_k=6656, ml_v=7680, ml_o=8704, ml_g=9728, merge=9760)


class KB:
    def __init__(self, nc, stack):
        self.nc = nc
        self.stack = stack
        self.eng = {'pe': nc.tensor, 'act': nc.scalar, 'dve': nc.vector,
                    'pool': nc.gpsimd, 'sp': nc.sync}
        self.esem = {}
        self.ecnt = {}
        for n in ['pe', 'act', 'dve', 'pool']:
            self.esem[n] = stack.enter_context(nc.semaphore('es_' + n))
            self.ecnt[n] = 0
        self.seen = {n: {} for n in self.eng}
        self.lastw = {}
        self.readers = {}
        self.dsem = {}
        self.dcnt = {}
        self.ninst = 0
        self.free = []
        self.nsem = 0

    def _wait(self, e, evs):
        need = {}
        for ev in evs:
            if ev is None:
                continue
            s, v = ev
            k = id(s)
            if e == 'pe' and s is self.esem['pe']:
                continue
            if self.seen[e].get(k, 0) >= v:
                continue
            if k not in need or need[k][1] < v:
                need[k] = (s, v)
        for k, (s, v) in need.items():
            self.eng[e].wait_ge(s, v)
            self.seen[e][k] = v

    def _deps(self, reads, writes):
        evs = []
        for r in reads:
            evs.append(self.lastw.get(r))
        for w in writes:
            evs.append(self.lastw.get(w))
            evs.extend(self.readers.get(w, ()))
        return evs

    def _commit(self, ev, reads, writes):
        for r in reads:
            self.readers.setdefault(r, []).append(ev)
        for w in writes:
            self.lastw[w] = ev
            self.readers[w] = []

    @staticmethod
    def _is_psum(r):
        return r == 'psb' or (isinstance(r, tuple) and len(r) == 2 and r[0] == 'ps')

    def op(self, e, fn, reads=(), writes=()):
        psr = [r for r in reads if self._is_psum(r)]
        if psr:
            writes = list(writes) + [r for r in psr if r not in writes]
        self._wait(e, self._deps(reads, writes))
        inst = fn()
        self.ecnt[e] += 1
        inst.then_inc(self.esem[e], 1)
        ev = (self.esem[e], self.ecnt[e])
        self._commit(ev, reads, writes)
        self.ninst += 1
        return ev

    def dma(self, q, out, in_, reads=(), writes=(), key=None, indirect=None, **kw):
        if key is None:
            key = writes[0] if writes else ('st', reads[0])
        if key not in self.dsem:
            if self.free:
                self.dsem[key], self.dcnt[key] = self.free.pop()
            else:
                self.nsem += 1
                self.dsem[key] = self.stack.enter_context(self.nc.semaphore('ds%d' % self.nsem))
                self.dcnt[key] = 0
        self._wait(q, self._deps(reads, writes))
        if indirect is not None:
            inst = self.nc.gpsimd.indirect_dma_start(out=out, in_=in_, **indirect)
        else:
            inst = self.eng[q].dma_start(out=out, in_=in_, **kw)
        self.dcnt[key] += 16
        inst.then_inc(self.dsem[key], 16)
        ev = (self.dsem[key], self.dcnt[key])
        self._commit(ev, reads, writes)
        self.ninst += 1
        return ev

    def barrier(self):
        evs = [(self.esem[n], self.ecnt[n]) for n in self.esem if self.ecnt[n] > 0]
        evs += [(self.dsem[k], self.dcnt[k]) for k in self.dsem]
        for e in ['pe', 'act', 'dve', 'pool', 'sp']:
            self._wait(e, evs)
        self.lastw = {}
        self.readers = {}
        for k in self.dsem:
            self.free.append((self.dsem[k], self.dcnt[k]))
        self.dsem = {}
        self.dcnt = {}


_UID = [0]


def sbt(nc, name, shape, dt):
    _UID[0] += 1
    return nc.sbuf_tensor('%s_u%d' % (name, _UID[0]), list(shape), dt)


class Rot:
    def __init__(self, nc, st, name, shape, dt, n):
        self.t = [st.enter_context(sbt(nc, '%s%d' % (name, i), list(shape), dt)) for i in range(n)]
        self.name = name
        self.i = 0

    def nxt(self):
        k = self.i % len(self.t)
        self.i += 1
        return self.t[k], (self.name, k)


def pipeline(n, load, comp, depth=1):
    for i in range(min(depth, n)):
        load(i)
    for i in range(n):
        if i + depth < n:
            load(i + depth)
        comp(i)


def build(SEQ, CTX, DEPTH, dbg=(), upto=None, proj_sel=None, small_moe=False):
    T = SEQ + CTX
    NCH = T // 128
    nc = bass.Bass("TRN2", target_bir_lowering=False)
    st0 = ExitStack()

    def din(name, shape, dt=F32):
        return nc.dram_tensor(name, list(shape), dt, kind="ExternalInput").ap()

    def dscr(name, shape, dt):
        kind = "ExternalOutput" if name in dbg else "Internal"
        return nc.dram_tensor(name, list(shape), dt, kind=kind).ap()

    L = DEPTH
    x_in = din('x', [SEQ, D]); ctx_in = din('ctx', [CTX, D])
    cc_in = din('cc', [128, KC, 2])
    w_mod = din('w_mod', [L, D, 6 * D]); b_modc = din('b_modc', [L, 128, 192])
    w_in = din('w_in', [L, D, 26144])
    aq_gain = din('att_q_gain', [L, 128]); ak_gain = din('att_k_gain', [L, 128])
    gm_ln_g = din('gm_ln_g', [L, BW]); gm_ln_b = din('gm_ln_b', [L, BW])
    gm_wsT = din('gm_wsT', [L, 8, 128, 128]); gm_b_s = din('gm_b_s', [L, 8 * 128])
    conv_wT = din('conv_wT', [L, BW, 31]); conv_bc = din('conv_bc', [L, 128, 8])
    conv_ln_gc = din('conv_ln_gc', [L, 128, 8]); conv_ln_bc = din('conv_ln_bc', [L, 128, 8])
    ml_gate_bias = din('ml_gate_bias', [L, 32]); ml_norm_gc = din('ml_norm_gc', [L, 128, 8])
    w_branch = din('w_branch', [L, 4, BW, D]); w_out = din('w_out', [L, D, D])
    ln1_g = din('ln1_g', [L, D]); ln1_b = din('ln1_b', [L, D])
    w_router = din('w_router', [L, D, NEXP])
    NE_DECL = 1 if small_moe else NEXP
    w_gate = din('w_gate', [L, NE_DECL, D, FF]); w_up = din('w_up', [L, NE_DECL, D, FF])
    w_down = din('w_down', [L, NE_DECL, FF, D])
    ln2_g = din('ln2_g', [L, D]); ln2_b = din('ln2_b', [L, D])
    cst = din('cst', [128, 8, 128])
    ropeC = din('ropeC', [SEQ, 64]); ropeS = din('ropeS', [SEQ, 64])
    iota_in = din('iota', [128, 1024]); tid_in = din('tid', [128, 64])
    zeros_in = din('zeros', [128, 4096])
    out_d = nc.dram_tensor('out', [SEQ, D], F32, kind="ExternalOutput").ap()

    XA = dscr('XA', [T, D], F32)
    NBLK = SEQ // 512 + (CTX + 511) // 512
    HT = dscr('HT', [NBLK, 128, KC, 512], BF16)
    GU = dscr('GU', [BW, T], BF16); GV = dscr('GV', [T, BW], F32)
    AQ = dscr('AQ', [T, BW], F32); AK = dscr('AK', [T, 256], F32); AV = dscr('AV', [T, 256], BF16)
    QT = dscr('QT', [BW, T], BF16); KT = dscr('KT', [256, T], BF16)
    CG = dscr('CG', [BW, T], F32)
    MQ = dscr('MQ', [BW, T], BF16); MK = dscr('MK', [BW, T], BF16); MKt = dscr('MKt', [T, BW], BF16)
    MV = dscr('MV', [T, BW], BF16); MO = dscr('MO', [BW, T], BF16); GT = dscr('GT', [T, 32], F32)
    BR = dscr('BR', [4, SEQ // 512 + (CTX + 511) // 512, 128, 8, 512], BF16)
    HF = dscr('HF', [BW, T], F32)
    YT = dscr('YT', [NBLK, 128, KC, 512], BF16)
    R1 = dscr('R1', [T, D], F32)
    XM = dscr('XM', [T, D], BF16)
    YM = [dscr('YM%d' % c_, [T, 512], F32) for c_ in range(8)]
    MODV = dscr('MODV', [L, 2, 6 * D], F32)

    kb = KB(nc, st0)
    PS = [st0.enter_context(nc.psum_tensor('ps%d' % i, [128, 512], F32)) for i in range(7)]
    PSB = st0.enter_context(nc.psum_tensor('psb', [128, 1024], BF16))
    psi = [0]
    bankset = [[0, 1, 2, 3, 4, 5, 6]]
    ALPHA = float((2 * 2) ** 0.25)

    def bank():
        k = bankset[0][psi[0] % len(bankset[0])]
        psi[0] += 1
        return PS[k], ('ps', k)

    CST = st0.enter_context(sbt(nc, 'CST', [128, 8, 128], F32))
    IDB = st0.enter_context(sbt(nc, 'IDB', [128, 128], BF16))
    ONEB = st0.enter_context(sbt(nc, 'ONEB', [128, 128], BF16))
    MODC = st0.enter_context(sbt(nc, 'MODC', [128, L, 192, 2], F32))
    MOD1P = st0.enter_context(sbt(nc, 'MOD1P', [128, L, 64, 2], F32))
    EPSC = st0.enter_context(sbt(nc, 'EPSC', [128, 1], F32))
    ONEC = st0.enter_context(sbt(nc, 'ONEC', [128, 1], F32))
    kb.op('pool', lambda: nc.gpsimd.memset(EPSC[:], EPS), [], ['EPSC'])
    kb.op('pool', lambda: nc.gpsimd.memset(ONEC[:], 1.0), [], ['ONEC'])
    kb.dma('sp', CST[:], cst, writes=['CST'])
    IDF = CST[:, 0, :]; ULE = CST[:, 1, :]; UGE = CST[:, 2, :]; ONEF = CST[:, 3, :]
    MLE = CST[:, 4, :]; MGE = CST[:, 5, :]; USTR = CST[:, 6, :]
    kb.op('dve', lambda: nc.vector.tensor_copy(out=IDB[:], in_=IDF), reads=['CST'], writes=['IDB'])
    kb.op('dve', lambda: nc.vector.tensor_copy(out=ONEB[:], in_=ONEF), reads=['CST'], writes=['ONEB'])

    streams = [('lat', 0, SEQ, 0), ('ctx', SEQ, CTX, 1)]
    blocks = []
    for (sn, u0, n, si) in streams:
        for b0 in range(0, n, 512):
            blocks.append((u0 + b0, min(512, n - b0), si))
    chunks = [(u, si) for (sn, u0, n, si) in streams for u in range(u0, u0 + n, 128)]

    def blk_of(u):
        if u < SEQ:
            return u // 512, u % 512
        return SEQ // 512 + (u - SEQ) // 512, (u - SEQ) % 512

    def act(out, in_, func, reads, writes, **kw):
        return kb.op('act', lambda: nc.scalar.activation(out=out, in_=in_, func=func, **kw), reads, writes)

    def tt(out, a, b, op, reads, writes, e='dve'):
        eng = nc.vector if e == 'dve' else nc.gpsimd
        return kb.op(e, lambda: eng.tensor_tensor(out=out, in0=a, in1=b, op=op), reads, writes)

    def ts(out, a, s1, s2, op0, op1, reads, writes, e='dve'):
        eng = nc.vector if e == 'dve' else nc.gpsimd
        if op1 is None:
            return kb.op(e, lambda: eng.tensor_scalar(out=out, in0=a, scalar1=s1, scalar2=None, op0=op0), reads, writes)
        return kb.op(e, lambda: eng.tensor_scalar(out=out, in0=a, scalar1=s1, scalar2=s2, op0=op0, op1=op1), reads, writes)

    def stt(out, a, s, b, op0, op1, reads, writes):
        return kb.op('dve', lambda: nc.vector.scalar_tensor_tensor(out=out, in0=a, scalar=s, in1=b, op0=op0, op1=op1), reads, writes)

    def cp(out, in_, reads, writes, e='dve'):
        eng = nc.vector if e == 'dve' else nc.gpsimd
        return kb.op(e, lambda: eng.tensor_copy(out=out, in_=in_), reads, writes)

    def mm(out, lhsT, rhs, start, stop, reads, writes):
        return kb.op('pe', lambda: nc.tensor.matmul(out, lhsT=lhsT, rhs=rhs, start=start, stop=stop), reads, writes)

    castsel = [0]

    def cast_load(dst3, src2, nk, res, stg):
        n = dst3.shape[-1]
        SE = stg.t[0].shape[1]
        kk = max(1, min(nk, SE // n))
        e = 'pool' if castsel[0] % 2 == 0 else 'act'
        castsel[0] += 1
        for k0 in range(0, nk, kk):
            k1 = min(nk, k0 + kk)
            s_t, s_r = stg.nxt()
            sv = s_t[:, 0:(k1 - k0) * n].rearrange('p (k n) -> p k n', n=n)
            kb.dma('sp', sv, src2[k0 * 128:k1 * 128, :].rearrange('(k p) n -> p k n', p=128), writes=[s_r])
            if e == 'pool':
                cp(dst3[:, k0:k1, :], sv, [s_r], [res], e='dve')
            else:
                act(dst3[:, k0:k1, :], sv, AF.Copy, [s_r], [res])

    def tr(out, in_, ident, reads, writes):
        return kb.op('pe', lambda: nc.tensor.transpose(out, in_, ident), reads, writes)

    for r0_ in range(0, SEQ, 512):
        kb.dma('sp', XA[r0_:r0_ + 512, :], x_in[r0_:r0_ + 512, :], key='cpx')
    kb.dma('sp', XA[SEQ:T, :], ctx_in, key='cpx')
    with ExitStack() as ph:
        sc = ph.enter_context(sbt(nc, 'sc', [128, KC, 2], F32))
        bm = ph.enter_context(sbt(nc, 'bm', [128, 192], F32))
        wr = Rot(nc, ph, 'wmod', [128, KC, 512], F32, 2)
        kb.dma('sp', sc[:], cc_in, writes=['sc'])
        act(sc[:], sc[:], AF.Silu, ['sc'], ['sc'])
        for l in range(L):
            kb.dma('sp', bm[:], b_modc[l], writes=['bm'])
            tiles = {}

            def ld(i, l=l):
                t, r = wr.nxt()
                tiles[i] = (t, r)
                kb.dma('sp', t[:], w_mod[l][:, i * 512:(i + 1) * 512].rearrange('(kc p) n -> p kc n', p=128), writes=[r])

            def cmp_(i, l=l):
                t, r = tiles.pop(i)
                for fc in range(4):
                    j = i * 4 + fc
                    p, pr = bank()
                    for kc in range(KC):
                        mm(p[:, 0:2], t[:, kc, fc * 128:(fc + 1) * 128], sc[:, kc, :], kc == 0, kc == KC - 1, [r, 'sc'], [pr])
                    ts(MODC[:, l, j, :], p[:, 0:2], bm[:, j:j + 1], None, ALU.add, None, [pr, 'bm'], ['MODC'])
            pipeline(48, ld, cmp_)
            ts(MOD1P[:, l, 0:32, :], MODC[:, l, 32:64, :], 1.0, None, ALU.add, None, ['MODC'], ['MOD1P'])
            ts(MOD1P[:, l, 32:64, :], MODC[:, l, 128:160, :], 1.0, None, ALU.add, None, ['MODC'], ['MOD1P'])
            for s in range(2):
                for (j0, nj) in [(0, 128), (128, 64)]:
                    tmp = ph.enter_context(sbt(nc, 'mt%d_%d_%d' % (l, s, j0), [128, 128], F32))
                    tmo = ph.enter_context(sbt(nc, 'mo%d_%d_%d' % (l, s, j0), [128, 128], F32))
                    rk = ('mt', l, s, j0)
                    cp(tmp[:, 0:nj], MODC[:, l, j0:j0 + nj, s], ['MODC'], [rk])
                    p, pr = bank()
                    tr(p[0:nj, 0:128], tmp[:, 0:nj], IDF, [rk, 'CST'], [pr])
                    cp(tmo[0:nj, :], p[0:nj, 0:128], [pr], [(rk, 'o')])
                    kb.dma('sp', MODV[l, s, j0 * 128:(j0 + nj) * 128].rearrange('(j p) -> j p', p=128), tmo[0:nj, :], reads=[(rk, 'o')])
    kb.barrier()

    def modcol(l, m, si):
        return MODC[:, l, m * 32:(m + 1) * 32, si]

    def gelu_tanh(out, src, srcres, outres, scr, w):
        s1, r1 = scr.nxt()
        act(s1[:, 0:w], src, AF.Square, srcres, [r1])
        ts(s1[:, 0:w], s1[:, 0:w], 0.044715, 1.0, ALU.mult, ALU.add, [r1], [r1])
        tt(s1[:, 0:w], s1[:, 0:w], src, ALU.mult, [r1] + srcres, [r1])
        act(s1[:, 0:w], s1[:, 0:w], AF.Sigmoid, [r1], [r1], scale=1.5957691216)
        tt(out, s1[:, 0:w], src, ALU.mult, [r1] + srcres, outres)

    def layer(l):
        last = (l == L - 1)
        with ExitStack() as ph:
            xr = Rot(nc, ph, 'xin', [128, D], F32, 2)
            hr = Rot(nc, ph, 'hto', [128, KC, 128], BF16, 2)
            tl = {}

            def ld(i):
                t, r = xr.nxt(); tl[i] = (t, r)
                u, si = chunks[i]
                kb.dma('sp', t[:], XA[u:u + 128, :], writes=[r])

            def cmp_(i):
                t, r = tl.pop(i)
                u, si = chunks[i]
                ho, hres = hr.nxt()
                for q in range(8):
                    p, pr = bank()
                    for k4 in range(4):
                        kc = q * 4 + k4
                        tr(p[:, k4 * 128:(k4 + 1) * 128], t[:, kc * 128:(kc + 1) * 128], IDF, [r, 'CST'], [pr])
                    for k4 in range(4):
                        kc = q * 4 + k4
                        act(ho[:, kc, :], p[:, k4 * 128:(k4 + 1) * 128], AF.Identity, [pr, 'MODC', 'MOD1P'], [hres],
                            scale=MOD1P[:, l, kc, si:si + 1], bias=MODC[:, l, kc, si:si + 1])
                bi_, bo_ = blk_of(u)
                kb.dma('sp', HT[bi_][:, :, bo_:bo_ + 128], ho[:], reads=[hres])
            pipeline(len(chunks), ld, cmp_)
        kb.barrier()
        if upto == 'ht':
            return False

        def linear(groups, wsrc_fn, act_src, nk, units):
            with ExitStack() as ph:
                wrot = Rot(nc, ph, 'lw', [128, nk, 512], BF16, 2)
                stg = Rot(nc, ph, 'lstg', [128, 4096], F32, 2)
                arot = Rot(nc, ph, 'la', [128, nk, 512], BF16, 2)
                scr = Rot(nc, ph, 'lscr', [128, 512], F32, 3)
                orot = Rot(nc, ph, 'lo', [128, 512], F32, 3)
                orotb = Rot(nc, ph, 'lob', [128, 512], BF16, 3)
                extra = units(ph) if units else None
                wt = {}

                def ldw(gi):
                    t, r = wrot.nxt(); wt[gi] = (t, r)
                    c = 0
                    for (c0, n) in groups[gi]['cols']:
                        cast_load(t[:, :, c:c + n], wsrc_fn(c0, n), nk, r, stg)
                        c += n

                def cmpg(gi):
                    g = groups[gi]
                    w_t, w_r = wt.pop(gi)
                    at = {}

                    def lda(bi):
                        t, r = arot.nxt(); at[bi] = (t, r)
                        u0, w, si = blocks[bi]
                        kb.dma('sp', t[:, :, 0:w], act_src[blk_of(u0)[0]][:, :, 0:w], writes=[r])

                    def cmpb(bi):
                        a_t, a_r = at.pop(bi)
                        u0, w, si = blocks[bi]
                        g['epi'](dict(w_t=w_t, w_r=w_r, a_t=a_t, a_r=a_r, u0=u0, w=w, si=si, scr=scr, orot=orot,
                                      orotb=orotb, extra=extra, nk=nk))
                    pipeline(len(blocks), lda, cmpb)
                pipeline(len(groups), ldw, cmpg)
            kb.barrier()

        def fmaj_unit(c, ci, nk):
            p, pr = bank()
            for kc in range(nk):
                mm(p[:, 0:c['w']], c['w_t'][:, kc, ci * 128:(ci + 1) * 128], c['a_t'][:, kc, 0:c['w']], kc == 0, kc == nk - 1,
                   [c['w_r'], c['a_r']], [pr])
            return p, pr

        def tmaj_unit(c, ti, ncols, nk):
            p, pr = bank()
            for kc in range(nk):
                mm(p[:, 0:ncols], c['a_t'][:, kc, ti * 128:(ti + 1) * 128], c['w_t'][:, kc, 0:ncols], kc == 0, kc == nk - 1,
                   [c['w_r'], c['a_r']], [pr])
            return p, pr

        def store(dst, src, res):
            kb.dma('sp', dst, src, reads=[res])

        def epi_F(kind, dstT, row0):
            def f(c):
                w = c['w']
                for ci in range(4):
                    p, pr = fmaj_unit(c, ci, c['nk'])
                    rows = slice(row0 + ci * 128, row0 + (ci + 1) * 128)
                    if kind == 'gelu':
                        o, orr = c['orotb'].nxt()
                        gelu_tanh(o[:, 0:w], p[:, 0:w], [pr], [orr], c['scr'], w)
                    elif kind == 'copy':
                        o, orr = c['orotb'].nxt()
                        cp(o[:, 0:w], p[:, 0:w], [pr], [orr])
                    elif kind == 'kscale':
                        o, orr = c['orotb'].nxt()
                        act(o[:, 0:w], p[:, 0:w], AF.Copy, [pr], [orr], scale=float(128 ** -0.5))
                    elif kind == 'sigmoid':
                        o, orr = c['orotb'].nxt()
                        act(o[:, 0:w], p[:, 0:w], AF.Sigmoid, [pr], [orr])
                    store(dstT[rows, c['u0']:c['u0'] + w], o[:, 0:w], orr)
            return f

        def epi_glu(row0):
            def f(c):
                w = c['w']
                for ci in range(2):
                    pa, pra = fmaj_unit(c, ci, c['nk'])
                    pb, prb = fmaj_unit(c, 2 + ci, c['nk'])
                    s, sr = c['scr'].nxt()
                    act(s[:, 0:w], pb[:, 0:w], AF.Sigmoid, [prb], [sr])
                    o, orr = c['orot'].nxt()
                    tt(o[:, 0:w], s[:, 0:w], pa[:, 0:w], ALU.mult, [sr, pra], [orr])
                    store(CG[row0 + ci * 128: row0 + (ci + 1) * 128, c['u0']:c['u0'] + w], o[:, 0:w], orr)
            return f

        def epi_T(kind, ncols, col_dst0):
            def f(c):
                w = c['w']
                for ti in range(w // 128):
                    p, pr = tmaj_unit(c, ti, ncols, c['nk'])
                    rows = slice(c['u0'] + ti * 128, c['u0'] + (ti + 1) * 128)
                    if kind == 'gv':
                        o, orr = c['orot'].nxt()
                        gelu_tanh(o[:, 0:ncols], p[:, 0:ncols], [pr], [orr], c['scr'], ncols)
                        store(GV[rows, col_dst0:col_dst0 + ncols], o[:, 0:ncols], orr)
                    elif kind == 'aq':
                        o, orr = c['orot'].nxt()
                        cp(o[:, 0:ncols], p[:, 0:ncols], [pr], [orr])
                        store(AQ[rows, col_dst0:col_dst0 + ncols], o[:, 0:ncols], orr)
                    elif kind == 'akv':
                        o, orr = c['orot'].nxt()
                        cp(o[:, 0:256], p[:, 0:256], [pr], [orr])
                        store(AK[rows, :], o[:, 0:256], orr)
                        ob, obr = c['orotb'].nxt()
                        act(ob[:, 0:256], p[:, 256:512], AF.Copy, [pr], [obr])
                        store(AV[rows, :], ob[:, 0:256], obr)
                    elif kind == 'mkt':
                        ob, obr = c['orotb'].nxt()
                        act(ob[:, 0:ncols], p[:, 0:ncols], AF.Copy, [pr], [obr], scale=float(128 ** -0.5))
                        store(MKt[rows, col_dst0:col_dst0 + ncols], ob[:, 0:ncols], obr)
                    elif kind == 'mv':
                        ob, obr = c['orotb'].nxt()
                        cp(ob[:, 0:ncols], p[:, 0:ncols], [pr], [obr])
                        store(MV[rows, col_dst0:col_dst0 + ncols], ob[:, 0:ncols], obr)
                    elif kind == 'gt':
                        o, orr = c['orot'].nxt()
                        cp(o[:, 0:32], p[:, 0:32], [pr], [orr])
                        store(GT[rows, :], o[:, 0:32], orr)
            return f

        groups = []
        for i in range(2):
            groups.append(dict(cols=[(OFF['gm_u'] + i * 512, 512)], epi=epi_F('gelu', GU, i * 512)))
            groups.append(dict(cols=[(OFF['ml_q'] + i * 512, 512)], epi=epi_F('copy', MQ, i * 512)))
            groups.append(dict(cols=[(OFF['ml_k'] + i * 512, 512)], epi=epi_F('kscale', MK, i * 512)))
            groups.append(dict(cols=[(OFF['ml_o'] + i * 512, 512)], epi=epi_F('sigmoid', MO, i * 512)))
            groups.append(dict(cols=[(OFF['gm_v'] + i * 512, 512)], epi=epi_T('gv', 512, i * 512)))
            groups.append(dict(cols=[(OFF['att_q'] + i * 512, 512)], epi=epi_T('aq', 512, i * 512)))
            groups.append(dict(cols=[(OFF['ml_k'] + i * 512, 512)], epi=epi_T('mkt', 512, i * 512)))
            groups.append(dict(cols=[(OFF['ml_v'] + i * 512, 512)], epi=epi_T('mv', 512, i * 512)))
        for i in range(4):
            groups.append(dict(cols=[(OFF['cv_a'] + i * 256, 256), (OFF['cv_b'] + i * 256, 256)], epi=epi_glu(i * 256)))
        groups.append(dict(cols=[(OFF['att_k'], 512)], epi=epi_T('akv', 512, 0)))
        groups.append(dict(cols=[(OFF['ml_g'], 32)], epi=epi_T('gt', 32, 0)))
        if proj_sel is not None:
            groups = [groups[v] for v in proj_sel]
        linear(groups, lambda c0, n: w_in[l][:, c0:c0 + n], HT, KC, None)
        if upto == 'proj':
            return False
        return PHASES_REST(l, last, linear, fmaj_unit, tmaj_unit, store, gelu_tanh)

    def PHASES_REST3(l, last, act_chunks, act_blocks):
        def ln_phase(src_fn, g_vec, b_vec, chlist, tag):
            with ExitStack() as ph:
                g_t = ph.enter_context(sbt(nc, tag + 'g', [128, D], F32))
                b_t = ph.enter_context(sbt(nc, tag + 'b', [128, D], F32))
                kb.dma('sp', g_t[:], g_vec.partition_broadcast(128), writes=['lnconst'])
                kb.dma('sp', b_t[:], b_vec.partition_broadcast(128), writes=['lnconst'])
                extra = src_fn(ph, None, None, None, init=True)
                xr = Rot(nc, ph, tag + 'x', [128, D], F32, 2)
                jk = Rot(nc, ph, tag + 'j', [128, D], BF16, 1)
                srot = Rot(nc, ph, tag + 's', [128, 8], F32, 3)
                tl = {}

                def ld(i):
                    u, si = chlist[i]
                    x, xres = xr.nxt(); tl[i] = (x, xres)
                    src_fn(ph, x, xres, (u, si), extra=extra, load=True)

                def cmp_(i):
                    u, si = chlist[i]
                    x, xres = tl.pop(i)
                    src_fn(ph, x, xres, (u, si), extra=extra, load=False)
                    j_, jres = jk.nxt()
                    ln_rows(x[:], D, xres, g_t[:], b_t[:], x[:], xres, srot, j_[:], jres)
                    kb.dma('sp', XA[u:u + 128, :], x[:], reads=[xres])
                pipeline(len(chlist), ld, cmp_)
            kb.barrier()

        def src_r1(ph, x, xres, cu, extra=None, load=True, init=False):
            if init:
                return None
            if load:
                kb.dma('sp', x[:], R1[cu[0]:cu[0] + 128, :], writes=[xres])
        ln_phase(src_r1, ln1_g[l], ln1_b[l], act_chunks, 'l1')
        if upto == 'ln1':
            return False

        for (sn, s0, ntok, si) in streams:
            if last and si == 1:
                continue
            NCs = ntok // 128
            cap = 2 * ntok // NEXP
            JS = min(128, cap); NJ = cap // JS
            with ExitStack() as mo:
                AFF = mo.enter_context(sbt(nc, 'e_aff', [128, NCs, NEXP], F32))
                TA = mo.enter_context(sbt(nc, 'e_ta', [128, NCs, NEXP, 2], F32))
                MASK = mo.enter_context(sbt(nc, 'e_mask', [128, NCs, NEXP], F32))
                SLOT = mo.enter_context(sbt(nc, 'e_slot', [128, NCs, NEXP], F32))
                OFFS = mo.enter_context(sbt(nc, 'e_offs', [128, NCs, NEXP], F32))
                TOT = mo.enter_context(sbt(nc, 'e_tot', [128, NCs, NEXP], F32))
                IDXF = mo.enter_context(sbt(nc, 'e_idxf', [128, NEXP, NJ, 2], F32))
                IDX = mo.enter_context(sbt(nc, 'e_idx', [128, NEXP, NJ], I32))
                IOTA = mo.enter_context(sbt(nc, 'e_iota', [128, 1024], F32))
                TID = mo.enter_context(sbt(nc, 'e_tid', [128, 64], F32))
                kb.dma('sp', IOTA[:], iota_in, writes=['e_iota'])
                kb.dma('sp', TID[:], tid_in, writes=['e_tid'])
                for c_ in range(8):
                    for r0 in range(s0, s0 + ntok, 128):
                        kb.dma('sp', YM[c_][r0:r0 + 128, :], zeros_in[:, 0:512], key='zym')
                with ExitStack() as ph:
                    s2p = ph.enter_context(sbt(nc, 'e_s2p', [128, D], F32))
                    sh2 = ph.enter_context(sbt(nc, 'e_sh2', [128, D], F32))
                    wr_ = ph.enter_context(sbt(nc, 'e_wr', [128, KC, NEXP], F32))
                    kb.dma('sp', s2p[:], MODV[l, si, 4 * D:5 * D].partition_broadcast(128), writes=['e_s2p'])
                    kb.dma('sp', sh2[:], MODV[l, si, 3 * D:4 * D].partition_broadcast(128), writes=['e_sh2'])
                    kb.dma('sp', wr_[:], w_router[l].rearrange('(kc p) e -> p kc e', p=128), writes=['e_wr'])
                    ts(s2p[:], s2p[:], 1.0, None, ALU.add, None, ['e_s2p'], ['e_s2p'], e='pool')
                    xr = Rot(nc, ph, 'e_x', [128, D], F32, 2)
                    xmr = Rot(nc, ph, 'e_xm', [128, D], F32, 1)
                    xbr = Rot(nc, ph, 'e_xb', [128, D], BF16, 2)
                    xtr = Rot(nc, ph, 'e_xt', [128, KC, 128], F32, 2)
                    smr = Rot(nc, ph, 'e_sm', [128, 24], F32, 2)
                    setbanks([0, 1, 2, 3, 4, 5])
                    tl = {}

                    def ld(i):
                        x, xres = xr.nxt(); tl[i] = (x, xres)
                        kb.dma('sp', x[:], XA[s0 + i * 128:s0 + (i + 1) * 128, :], writes=[xres])

                    def cmp_(i):
                        x, xres = tl.pop(i)
                        xm, xmres = xmr.nxt(); xb, xbres = xbr.nxt()
                        tt(xm[:], x[:], s2p[:], ALU.mult, [xres, 'e_s2p'], [xmres])
                        tt(xb[:], xm[:], sh2[:], ALU.add, [xmres, 'e_sh2'], [xbres], e='pool')
                        kb.dma('sp', XM[s0 + i * 128:s0 + (i + 1) * 128, :], xb[:], reads=[xbres])
                        xt, xtres = xtr.nxt()
                        for q in range(8):
                            p, pr = bank()
                            for k4 in range(4):
                                kc = q * 4 + k4
                                tr(p[:, k4 * 128:(k4 + 1) * 128], x[:, kc * 128:(kc + 1) * 128], IDF, [xres, 'CST'], [pr])
                            for k4 in range(4):
                                kc = q * 4 + k4
                                act(xt[:, kc, :], p[:, k4 * 128:(k4 + 1) * 128], AF.Identity, [pr, 'MODC', 'MOD1P'], [xtres],
                                    scale=MOD1P[:, l, 32 + kc, si:si + 1], bias=MODC[:, l, 96 + kc, si:si + 1])
                        pl, plr = PS[6], ('ps', 6)
                        for kc in range(KC):
                            mm(pl[:, 0:NEXP], xt[:, kc, :], wr_[:, kc, :], kc == 0, kc == KC - 1, [xtres, 'e_wr'], [plr])
                        sm, smres = smr.nxt()
                        kb.op('dve', lambda: nc.vector.tensor_reduce(out=sm[:, 0:1], in_=pl[:, 0:NEXP], axis=AX.X, op=ALU.max), [plr], [smres])
                        ts(sm[:, 1:2], sm[:, 0:1], -1.0, None, ALU.mult, None, [smres], [smres])
                        act(sm[:, 8:24], pl[:, 0:NEXP], AF.Exp, [plr, smres], [smres], bias=sm[:, 1:2], accum_out=sm[:, 2:3])
                        kb.op('dve', lambda: nc.vector.reciprocal(out=sm[:, 3:4], in_=sm[:, 2:3]), [smres], [smres])
                        ts(AFF[:, i, :], sm[:, 8:24], sm[:, 3:4], None, ALU.mult, None, [smres], ['e_aff'])
                    pipeline(NCs, ld, cmp_)
                kb.barrier()
                with ExitStack() as ph:
                    lo = ph.enter_context(sbt(nc, 'b_lo', [128, NEXP], F32))
                    hi = ph.enter_context(sbt(nc, 'b_hi', [128, NEXP], F32))
                    mid = ph.enter_context(sbt(nc, 'b_mid', [128, NEXP], F32))
                    ge = ph.enter_context(sbt(nc, 'b_ge', [128, NEXP], F32))
                    d1 = ph.enter_context(sbt(nc, 'b_d1', [128, NEXP], F32))
                    cntp = ph.enter_context(sbt(nc, 'b_cnt', [128, NEXP], F32))
                    cmpt = ph.enter_context(sbt(nc, 'b_cmp', [128, NCs, NEXP], F32))
                    kb.op('pool', lambda: nc.gpsimd.memset(lo[:], 0.0), [], ['b_lo'])
                    kb.op('pool', lambda: nc.gpsimd.memset(hi[:], 1.0), [], ['b_hi'])
                    for it in range(36):
                        tt(mid[:], lo[:], hi[:], ALU.add, ['b_lo', 'b_hi'], ['b_mid'])
                        ts(mid[:], mid[:], 0.5, None, ALU.mult, None, ['b_mid'], ['b_mid'])
                        tt(cmpt[:], AFF[:], mid[:].unsqueeze(1).broadcast_to([128, NCs, NEXP]), ALU.is_ge, ['e_aff', 'b_mid'], ['b_cmp'])
                        kb.op('dve', lambda: nc.vector.tensor_reduce(out=cntp[:], in_=cmpt[:].rearrange('p n e -> p e n'), axis=AX.X, op=ALU.add), ['b_cmp'], ['b_cnt'])
                        pc, pcr = PS[it % 4], ('ps', it % 4)
                        mm(pc[:, 0:NEXP], ONEF, cntp[:], True, True, ['CST', 'b_cnt'], [pcr])
                        ts(ge[:], pc[:, 0:NEXP], float(cap) - 0.5, None, ALU.is_ge, None, [pcr], ['b_ge'])
                        tt(d1[:], mid[:], lo[:], ALU.subtract, ['b_mid', 'b_lo'], ['b_d1'])
                        tt(d1[:], d1[:], ge[:], ALU.mult, ['b_d1', 'b_ge'], ['b_d1'])
                        tt(lo[:], lo[:], d1[:], ALU.add, ['b_lo', 'b_d1'], ['b_lo'])
                        tt(d1[:], hi[:], mid[:], ALU.subtract, ['b_hi', 'b_mid'], ['b_d1'])
                        tt(d1[:], d1[:], ge[:], ALU.mult, ['b_d1', 'b_ge'], ['b_d1'])
                        tt(hi[:], mid[:], d1[:], ALU.add, ['b_mid', 'b_d1'], ['b_hi'])
                    tt(MASK[:], AFF[:], lo[:].unsqueeze(1).broadcast_to([128, NCs, NEXP]), ALU.is_ge, ['e_aff', 'b_lo'], ['e_mask'])
                    NE = NCs * NEXP
                    Mf = MASK[:].rearrange('p n e -> p (n e)')
                    Sf = SLOT[:].rearrange('p n e -> p (n e)')
                    Tf = TOT[:].rearrange('p n e -> p (n e)')
                    for c0 in range(0, NE, 512):
                        c1 = min(NE, c0 + 512)
                        p1, p1r = PS[4], ('ps', 4)
                        p2, p2r = PS[5], ('ps', 5)
                        mm(p1[:, 0:c1 - c0], USTR, Mf[:, c0:c1], True, True, ['CST', 'e_mask'], [p1r])
                        mm(p2[:, 0:c1 - c0], ONEF, Mf[:, c0:c1], True, True, ['CST', 'e_mask'], [p2r])
                        cp(Sf[:, c0:c1], p1[:, 0:c1 - c0], [p1r], ['e_slot'])
                        cp(Tf[:, c0:c1], p2[:, 0:c1 - c0], [p2r], ['e_tot'])
                    kb.op('pool', lambda: nc.gpsimd.memset(OFFS[:, 0, :], 0.0), [], ['e_offs'])
                    for n in range(1, NCs):
                        tt(OFFS[:, n, :], OFFS[:, n - 1, :], TOT[:, n - 1, :], ALU.add, ['e_offs', 'e_tot'], ['e_offs'])
                    tt(SLOT[:], SLOT[:], OFFS[:], ALU.add, ['e_slot', 'e_offs'], ['e_slot'])
                    ts(SLOT[:], SLOT[:], 1.0, None, ALU.add, None, ['e_slot'], ['e_slot'])
                    tt(SLOT[:], SLOT[:], MASK[:], ALU.mult, ['e_slot', 'e_mask'], ['e_slot'])
                    ts(SLOT[:], SLOT[:], -1.0, None, ALU.add, None, ['e_slot'], ['e_slot'])
                    for n in range(NCs):
                        ts(TA[:, n, :, 0], AFF[:, n, :], 0.0, TID[:, n:n + 1], ALU.mult, ALU.add, ['e_aff', 'e_tid'], ['e_ta'], e='pool')
                    ts(TA[:, :, :, 0], TA[:, :, :, 0], float(s0), None, ALU.add, None, ['e_ta'], ['e_ta'], e='pool')
                    cp(TA[:, :, :, 1], AFF[:], ['e_aff'], ['e_ta'], e='pool')
                    selr = Rot(nc, ph, 'e_sel', [128, 128], F32, 3)
                    setbanks([0, 1, 2, 3])
                    for e in range(NEXP):
                        for jc in range(NJ):
                            pi, pir = bank()
                            for n in range(NCs):
                                sl, slres = selr.nxt()
                                ts(sl[:, 0:JS], IOTA[:, jc * JS:(jc + 1) * JS], SLOT[:, n, e:e + 1], None, ALU.is_equal, None, ['e_iota', 'e_slot'], [slres])
                                mm(pi[0:JS, 0:2], sl[:, 0:JS], TA[:, n, e, :], n == 0, n == NCs - 1, [slres, 'e_ta'], [pir])
                            cp(IDXF[0:JS, e, jc, :], pi[0:JS, 0:2], [pir], ['e_idxf'])
                    ts(IDXF[0:JS, :, :, 0], IDXF[0:JS, :, :, 0], 0.25, None, ALU.add, None, ['e_idxf'], ['e_idxf'])
                    cp(IDX[0:JS, :, :], IDXF[0:JS, :, :, 0], ['e_idxf'], ['e_idx'])
                kb.barrier()
                with ExitStack() as ph:
                    xer = Rot(nc, ph, 'e_xe', [128, D], BF16, 1)
                    stg = Rot(nc, ph, 'e_stg', [128, 2048], F32, 2)
                    xeT = ph.enter_context(sbt(nc, 'e_xeT', [128, KC, cap], BF16))
                    wgr = Rot(nc, ph, 'e_wg', [128, KC, 128], BF16, 2)
                    wur = Rot(nc, ph, 'e_wu', [128, KC, 128], BF16, 2)
                    hidT = ph.enter_context(sbt(nc, 'e_hid', [128, 8, cap], BF16))
                    wdr = Rot(nc, ph, 'e_wd', [128, 8, 512], BF16, 2)
                    sgr = Rot(nc, ph, 'e_sg', [128, 512], F32, 2)
                    yer = Rot(nc, ph, 'e_ye', [128, 512], F32, 3)
                    setbanks([0, 1, 2, 3, 4, 5])
                    for e in range(NEXP):
                        for jc in range(NJ):
                            xe, xeres = xer.nxt()
                            kb.dma('pool', xe[0:JS, :], XM[:, :], writes=[xeres], reads=['e_idx'],
                                   indirect=dict(out_offset=None, in_offset=bass.IndirectOffsetOnAxis(ap=IDX[0:JS, e, jc:jc + 1], axis=0)))
                            for k8 in range(4):
                                for kk in range(8):
                                    kc = k8 * 8 + kk
                                    tr(PSB[:, kk * 128:kk * 128 + JS], xe[0:JS, kc * 128:(kc + 1) * 128], IDB[0:JS, 0:JS], [xeres, 'IDB'], ['psb'])
                                cp(xeT[:, k8 * 8:(k8 + 1) * 8, jc * JS:(jc + 1) * JS],
                                   PSB[:, :].rearrange('p (k t) -> p k t', t=128)[:, :, 0:JS], ['psb'], ['e_xeT'])
                        wl = {}

                        def ldw(fc, e=e):
                            wg, wgres = wgr.nxt(); wu, wures = wur.nxt()
                            wl[fc] = (wg, wgres, wu, wures)
                            cast_load(wg[:], w_gate[l][e][:, fc * 128:(fc + 1) * 128], KC, wgres, stg)
                            cast_load(wu[:], w_up[l][e][:, fc * 128:(fc + 1) * 128], KC, wures, stg)

                        def cmpw(fc):
                            wg, wgres, wu, wures = wl.pop(fc)
                            for j0 in range(0, cap, 512):
                                jw = min(512, cap - j0)
                                pg, pgr = bank(); pu, pur = bank()
                                for kc in range(KC):
                                    mm(pg[:, 0:jw], wg[:, kc, :], xeT[:, kc, j0:j0 + jw], kc == 0, kc == KC - 1, [wgres, 'e_xeT'], [pgr])
                                for kc in range(KC):
                                    mm(pu[:, 0:jw], wu[:, kc, :], xeT[:, kc, j0:j0 + jw], kc == 0, kc == KC - 1, [wures, 'e_xeT'], [pur])
                                sg, sgres = sgr.nxt()
                                act(sg[:, 0:jw], pg[:, 0:jw], AF.Silu, [pgr], [sgres])
                                tt(hidT[:, fc, j0:j0 + jw], sg[:, 0:jw], pu[:, 0:jw], ALU.mult, [sgres, pur], ['e_hid'])
                        pipeline(8, ldw, cmpw)
                        dl = {}

                        def ldd(ct, e=e):
                            wd, wdres = wdr.nxt(); dl[ct] = (wd, wdres)
                            cast_load(wd[:], w_down[l][e][:, ct * 512:(ct + 1) * 512], 8, wdres, stg)

                        def cmpd(ct, e=e):
                            wd, wdres = dl.pop(ct)
                            for jc in range(NJ):
                                p, pr = bank()
                                for fc in range(8):
                                    mm(p[0:JS, :], hidT[:, fc, jc * JS:(jc + 1) * JS], wd[:, fc, :], fc == 0, fc == 7, ['e_hid', wdres], [pr])
                                ye, yeres = yer.nxt()
                                ts(ye[0:JS, :], p[0:JS, :], IDXF[0:JS, e, jc, 1:2], None, ALU.mult, None, [pr, 'e_idxf'], [yeres])
                                kb.dma('pool', YM[ct][:, :], ye[0:JS, :], reads=[yeres, 'e_idx'], writes=['YMd'], key=('sc', yeres),
                                       indirect=dict(out_offset=bass.IndirectOffsetOnAxis(ap=IDX[0:JS, e, jc:jc + 1], axis=0), in_offset=None,
                                                     compute_op=ALU.add))
                        pipeline(8, ldd, cmpd)
                kb.barrier()

        def src_x2(ph, x, xres, cu, extra=None, load=True, init=False):
            if init:
                g2 = ph.enter_context(sbt(nc, 'l2_g2', [128, 2, D], F32))
                for s_ in range(2):
                    kb.dma('sp', g2[:, s_, :], MODV[l, s_, 5 * D:6 * D].partition_broadcast(128), writes=['l2_g2'])
                return dict(g2=g2, ym=Rot(nc, ph, 'l2_ym', [128, D], F32, 2), cur={})
            u, si = cu
            if load:
                ym, ymres = extra['ym'].nxt()
                extra['cur'][u] = (ym, ymres)
                kb.dma('sp', x[:], XA[u:u + 128, :], writes=[xres])
                for c_ in range(8):
                    kb.dma('sp', ym[:, c_ * 512:(c_ + 1) * 512], YM[c_][u:u + 128, :], writes=[ymres])
            else:
                ym, ymres = extra['cur'].pop(u)
                tt(ym[:], ym[:], extra['g2'][:, si, :], ALU.mult, [ymres, 'l2_g2'], [ymres])
                stt(x[:], x[:], ALPHA, ym[:], ALU.mult, ALU.add, [xres, ymres], [xres])
        ln_phase(src_x2, ln2_g[l], ln2_b[l], act_chunks, 'l2')
        return True

    def PHASES_REST2(l, last, linear, fmaj_unit, tmaj_unit, store, act_chunks, act_blocks):
        with ExitStack() as ph:
            G = ph.enter_context(sbt(nc, 'm_G', [128, NCH, 32], F32))
            LS = ph.enter_context(sbt(nc, 'm_LS', [128, NCH, 32], F32))
            TMPG = ph.enter_context(sbt(nc, 'm_TG', [128, NCH, 32], F32))
            GB = ph.enter_context(sbt(nc, 'm_GB', [128, 32], F32))
            Bc = ph.enter_context(sbt(nc, 'm_Bc', [128, NCH, 2, 8], F32))
            Bt = ph.enter_context(sbt(nc, 'm_Bt', [128, NCH, 2, 8], F32))
            BI = ph.enter_context(sbt(nc, 'm_BI', [128, NCH, 2, 8], F32))
            WS = ph.enter_context(sbt(nc, 'm_WS', [128, NCH, 2, 8], F32))
            AT = ph.enter_context(sbt(nc, 'm_AT', [128, NCH, 2, 8], F32))
            NG = ph.enter_context(sbt(nc, 'm_NG', [128, 8], F32))
            for n0_ in range(0, NCH, 16):
                n1_ = min(NCH, n0_ + 16)
                kb.dma('sp', G[:, n0_:n1_, :], GT[n0_ * 128:n1_ * 128, :].rearrange('(n p) g -> p n g', p=128), writes=['mG'])
            kb.dma('sp', GB[:], ml_gate_bias[l].partition_broadcast(128), writes=['mGB'])
            kb.dma('sp', NG[:], ml_norm_gc[l], writes=['mNG'])
            tt(G[:], G[:], GB[:].unsqueeze(1).broadcast_to([128, NCH, 32]), ALU.add, ['mG', 'mGB'], ['mG'])
            act(TMPG[:], G[:], AF.Abs, ['mG'], ['mTG'])
            act(TMPG[:], TMPG[:], AF.Exp, ['mTG'], ['mTG'], scale=-1.0)
            act(TMPG[:], TMPG[:], AF.Ln, ['mTG'], ['mTG'], bias=ONEC[:, 0:1])
            ts(LS[:], G[:], 0.0, None, ALU.min, None, ['mG'], ['mLS'])
            tt(LS[:], LS[:], TMPG[:], ALU.subtract, ['mLS', 'mTG'], ['mLS'])
            G4 = G[:].rearrange('p n (a h) -> p n a h', a=4)
            L4 = LS[:].rearrange('p n (a h) -> p n a h', a=4)
            setbanks([0, 1, 2, 3, 4, 5, 6])
            for n in range(NCH):
                p, pr = bank()
                for dr in range(2):
                    U = ULE if dr == 0 else UGE
                    mm(p[:, dr * 8:dr * 8 + 8], U, L4[:, n, 1 + 2 * dr, :], True, True, ['CST', 'mLS'], [pr])
                    mm(p[:, 16 + dr * 8:16 + dr * 8 + 8], ONEF, L4[:, n, 1 + 2 * dr, :], True, True, ['CST', 'mLS'], [pr])
                cp(Bc[:, n, :, :], p[:, 0:16].rearrange('p (a h) -> p a h', a=2), [pr], ['mBc'])
                cp(Bt[:, n, :, :], p[:, 16:32].rearrange('p (a h) -> p a h', a=2), [pr], ['mBt'])
            for dr in range(2):
                tt(BI[:, :, dr, :], G4[:, :, 2 * dr, :], Bc[:, :, dr, :], ALU.subtract, ['mG', 'mBc'], ['mBI'])
            tt(WS[:], Bt[:], BI[:], ALU.add, ['mBt', 'mBI'], ['mWS'])
            act(WS[:], WS[:], AF.Exp, ['mWS'], ['mWS'])
            act(AT[:], Bt[:], AF.Exp, ['mBt'], ['mAT'])

            qTr = Rot(nc, ph, 'm_q', [128, 8, 128], BF16, 2)
            kTr = Rot(nc, ph, 'm_k', [128, 8, 128], BF16, 2)
            ktr = Rot(nc, ph, 'm_kt', [128, BW], BF16, 2)
            vxr = Rot(nc, ph, 'm_vx', [128, 8, 129], BF16, 2)
            hfr = Rot(nc, ph, 'm_hf', [128, 8, 128], F32, 2)
            mor = Rot(nc, ph, 'm_mo', [128, 8, 128], BF16, 2)
            outr = Rot(nc, ph, 'm_out', [128, 8, 128], F32, 2)
            outb = Rot(nc, ph, 'm_outb', [128, 8, 128], BF16, 2)
            lfb = Rot(nc, ph, 'm_lfb', [128, 128], F32, 2)
            ejr = Rot(nc, ph, 'm_ej', [128, 128], F32, 2)
            ar_ = Rot(nc, ph, 'm_a', [128, 128], F32, 2)
            pr_ = Rot(nc, ph, 'm_p', [128, 128], BF16, 2)
            qsr = Rot(nc, ph, 'm_qs', [128, 128], BF16, 2)
            kwr = Rot(nc, ph, 'm_kw', [128, 128], BF16, 2)
            dnr = Rot(nc, ph, 'm_dn', [128, 128], F32, 2)
            hdr = Rot(nc, ph, 'm_hd', [128, 128], F32, 2)
            sqr = Rot(nc, ph, 'm_sq', [128, 128], F32, 2)
            str_ = Rot(nc, ph, 'm_st', [128, 3, 128], F32, 2)
            Cst = ph.enter_context(sbt(nc, 'm_C', [128, 8, 129], F32))
            Cb = ph.enter_context(sbt(nc, 'm_Cb', [128, 8, 128], BF16))
            Nrep = ph.enter_context(sbt(nc, 'm_N', [128, 8, 128], BF16))
            for t_, r_ in zip(vxr.t, range(2)):
                kb.op('pool', lambda: nc.gpsimd.memset(t_[:], 1.0), [], [('m_vx', r_)])
            lat_ch = list(range(0, SEQ // 128)); ctx_ch = list(range(SEQ // 128, NCH))
            for dr in range(2):
                U = ULE if dr == 0 else UGE
                MSK = MLE if dr == 0 else MGE
                kb.op('pool', lambda: nc.gpsimd.memset(Cst[:], 0.0), [], [('mC', h) for h in range(8)])
                kb.op('pool', lambda: nc.gpsimd.memset(Cb[:], 0.0), [], [('mCb', h) for h in range(8)])
                kb.op('pool', lambda: nc.gpsimd.memset(Nrep[:], 0.0), [], [('mCb', h) for h in range(8)])
                order = (ctx_ch + lat_ch) if dr == 0 else (ctx_ch[::-1] + lat_ch[::-1])
                tl = {}

                def ld(i, dr=dr, order=order):
                    n = order[i]; u = n * 128
                    q, qres = qTr.nxt(); k, kres = kTr.nxt(); kt, ktres = ktr.nxt(); vx, vxres = vxr.nxt()
                    ent = [q, qres, k, kres, kt, ktres, vx, vxres]
                    kb.dma('sp', q[:], MQ.rearrange('(h d) t -> d h t', d=128)[:, :, u:u + 128], writes=[qres])
                    kb.dma('sp', k[:], MK.rearrange('(h d) t -> d h t', d=128)[:, :, u:u + 128], writes=[kres])
                    kb.dma('sp', kt[:], MKt[u:u + 128, :], writes=[ktres])
                    kb.dma('sp', vx[:, :, 0:128], MV[u:u + 128, :].rearrange('t (h e) -> t h e', e=128), writes=[vxres])
                    if dr == 1:
                        hf, hfres = hfr.nxt(); mo, mores = mor.nxt()
                        kb.dma('sp', hf[:], HF.rearrange('(h e) t -> e h t', e=128)[:, :, u:u + 128], writes=[hfres])
                        kb.dma('sp', mo[:], MO.rearrange('(h e) t -> e h t', e=128)[:, :, u:u + 128], writes=[mores])
                        ent += [hf, hfres, mo, mores]
                    tl[i] = ent

                def cmp_(i, dr=dr, order=order, U=U, MSK=MSK):
                    n = order[i]; u = n * 128
                    ent = tl.pop(i)
                    q, qres, k, kres, kt, ktres, vx, vxres = ent[:8]
                    if dr == 0:
                        ot, otres = outr.nxt()
                    else:
                        hf, hfres, mo, mores = ent[8:]
                        ob, obres = outb.nxt()
                    for h in range(8):
                        lf = L4[:, n, 1 + 2 * dr, h:h + 1]
                        lb_, lbres = lfb.nxt()
                        act(lb_[:], ONEF, AF.Copy, ['CST', 'mLS'], [lbres], scale=lf)
                        pb, pbr = PS[0], ('ps', 0)
                        mm(pb[:, 0:128], lb_[:], U, True, True, [lbres, 'CST'], [pbr])
                        ej, ejres = ejr.nxt()
                        act(ej[:], pb[:, 0:128], AF.Exp, [pbr], [ejres])
                        a_, ares = ar_.nxt()
                        act(a_[:], pb[:, 0:128], AF.Exp, [pbr, 'mBI'], [ares], bias=BI[:, n, dr, h:h + 1])
                        tt(a_[:], a_[:], MSK, ALU.mult, [ares, 'CST'], [ares], e='pool')
                        pst, pstr = PS[1], ('ps', 1)
                        mm(pst[:, 0:128], k[:, h, :], q[:, h, :], True, True, [kres, qres], [pstr])
                        p_, pres = pr_.nxt()
                        tt(p_[:], pst[:, 0:128], a_[:], ALU.mult, [pstr, ares], [pres])
                        qs, qsres = qsr.nxt()
                        tt(qs[:], q[:, h, :], ej[:], ALU.mult, [qres, ejres], [qsres], e='pool')
                        pn, pnr = PS[2], ('ps', 2)
                        mm(pn[:, 0:128], vx[:, h, 0:128], p_[:], True, False, [vxres, pres], [pnr])
                        mm(pn[:, 0:128], Cb[:, h, :], qs[:], False, True, [('mCb', h), qsres], [pnr])
                        pd, pdr = PS[3], ('ps', 3)
                        mm(pd[:, 0:128], ONEB[:], p_[:], True, False, ['ONEB', pres], [pdr])
                        mm(pd[:, 0:128], Nrep[:, h, :], qs[:], False, True, [('mCb', h), qsres], [pdr])
                        dn, dnres = dnr.nxt()
                        act(dn[:], pd[:, 0:128], AF.Abs, [pdr], [dnres])
                        ts(dn[:], dn[:], 1.0, None, ALU.max, None, [dnres], [dnres])
                        kb.op('dve', lambda: nc.vector.reciprocal(out=dn[:], in_=dn[:]), [dnres], [dnres])
                        if dr == 0:
                            tt(ot[:, h, :], pn[:, 0:128], dn[:], ALU.mult, [pnr, dnres], [otres])
                        else:
                            hd, hdres = hdr.nxt()
                            tt(hd[:], pn[:, 0:128], dn[:], ALU.mult, [pnr, dnres], [hdres])
                            tt(hd[:], hd[:], hf[:, h, :], ALU.add, [hdres, hfres], [hdres])
                            sq, sqres = sqr.nxt()
                            act(sq[:], hd[:], AF.Square, [hdres], [sqres])
                            pm, pmr = PS[5], ('ps', 5)
                            pq, pqr = PS[6], ('ps', 6)
                            mm(pm[:, 0:128], ONEF, hd[:], True, True, ['CST', hdres], [pmr])
                            mm(pq[:, 0:128], ONEF, sq[:], True, True, ['CST', sqres], [pqr])
                            s_, sres = str_.nxt()
                            ts(s_[:, 0, :], pm[:, 0:128], 1.0 / 128, None, ALU.mult, None, [pmr], [sres])
                            tt(s_[:, 1, :], s_[:, 0, :], s_[:, 0, :], ALU.mult, [sres], [sres])
                            stt(s_[:, 2, :], pq[:, 0:128], 1.0 / 128, s_[:, 1, :], ALU.mult, ALU.subtract, [pqr, sres], [sres])
                            act(s_[:, 2, :], s_[:, 2, :], AF.Sqrt, [sres], [sres], bias=EPSC[:, 0:1])
                            kb.op('dve', lambda: nc.vector.reciprocal(out=s_[:, 1, :], in_=s_[:, 2, :]), [sres], [sres])
                            tt(hd[:], hd[:], s_[:, 0, :], ALU.subtract, [hdres, sres], [hdres])
                            tt(hd[:], hd[:], s_[:, 1, :], ALU.mult, [hdres, sres], [hdres])
                            stt(ob[:, h, :], hd[:], NG[:, h:h + 1], mo[:, h, :], ALU.mult, ALU.mult, [hdres, 'mNG', mores], [obres])
                        kw, kwres = kwr.nxt()
                        act(kw[:], kt[:, h * 128:(h + 1) * 128], AF.Copy, [ktres, 'mWS'], [kwres], scale=WS[:, n, dr, h:h + 1])
                        pc, pcr = PS[4], ('ps', 4)
                        mm(pc[:, 0:129], kw[:], vx[:, h, :], True, True, [kwres, vxres], [pcr])
                        stt(Cst[:, h, :], Cst[:, h, :], AT[:, n, dr, h:h + 1], pc[:, 0:129], ALU.mult, ALU.add, [('mC', h), 'mAT', pcr], [('mC', h)])
                        cp(Cb[:, h, :], Cst[:, h, 0:128], [('mC', h)], [('mCb', h)], e='pool')
                        act(Nrep[:, h, :], ONEF, AF.Copy, ['CST', ('mC', h)], [('mCb', h)], scale=Cst[:, h, 128:129])
                    if dr == 0:
                        kb.dma('sp', HF.rearrange('(h e) t -> e h t', e=128)[:, :, u:u + 128], ot[:], reads=[otres])
                    else:
                        bi_, bo_ = blk_of(u)
                        kb.dma('sp', BR[3][bi_][:, :, bo_:bo_ + 128], ob[:], reads=[obres])
                pipeline(len(order), ld, cmp_)
                kb.barrier()
        kb.barrier()
        if upto == 'mlstm':
            return False

        with ExitStack() as ph:
            wgr = Rot(nc, ph, 'mg_wg', [128, KC, 4, 128], BF16, 1)
            wbr = Rot(nc, ph, 'mg_wb', [128, 8, 4, 128], BF16, 1)
            hr = Rot(nc, ph, 'mg_h', [128, KC, 512], BF16, 2)
            brr = Rot(nc, ph, 'mg_b', [128, 4, 8, 512], BF16, 2)
            sgr = Rot(nc, ph, 'mg_sg', [128, 512], F32, 2)
            stg = Rot(nc, ph, 'mg_stg', [128, 2048], F32, 2)
            accr = Rot(nc, ph, 'mg_acc', [128, 512], F32, 2)
            yor = Rot(nc, ph, 'mg_yo', [128, 512], BF16, 2)
            setbanks([0, 1, 2, 3, 4, 5])
            wl = {}

            def ldw(nch):
                wg, wgres = wgr.nxt(); wb, wbres = wbr.nxt()
                wl[nch] = (wg, wgres, wb, wbres)
                for i in range(4):
                    c0 = OFF['merge'] + i * D + nch * 128
                    cast_load(wg[:, :, i, :], w_in[l][:, c0:c0 + 128], KC, wgres, stg)
                    cast_load(wb[:, :, i, :], w_branch[l][i][:, nch * 128:(nch + 1) * 128], 8, wbres, stg)

            def cmpw(nch):
                wg, wgres, wb, wbres = wl.pop(nch)
                al = {}

                def lda(bi):
                    u0, w, si = act_blocks[bi]
                    h_, hres = hr.nxt(); b_, bres = brr.nxt()
                    al[bi] = (h_, hres, b_, bres)
                    kb.dma('sp', h_[:, :, 0:w], HT[blk_of(u0)[0]][:, :, 0:w], writes=[hres])
                    for i in range(4):
                        kb.dma('sp', b_[:, i, :, 0:w], BR[i][blk_of(u0)[0]][:, :, 0:w], writes=[bres])

                def cmpa(bi):
                    u0, w, si = act_blocks[bi]
                    h_, hres, b_, bres = al.pop(bi)
                    acc, accres = accr.nxt()
                    for i in range(4):
                        pg, pgr = bank()
                        for kc in range(KC):
                            mm(pg[:, 0:w], wg[:, kc, i, :], h_[:, kc, 0:w], kc == 0, kc == KC - 1, [wgres, hres], [pgr])
                        pb, pbr = bank()
                        for kc in range(8):
                            mm(pb[:, 0:w], wb[:, kc, i, :], b_[:, i, kc, 0:w], kc == 0, kc == 7, [wbres, bres], [pbr])
                        sg, sgres = sgr.nxt()
                        act(sg[:, 0:w], pg[:, 0:w], AF.Sigmoid, [pgr], [sgres])
                        if i == 0:
                            tt(acc[:, 0:w], sg[:, 0:w], pb[:, 0:w], ALU.mult, [sgres, pbr], [accres])
                        else:
                            tt(sg[:, 0:w], sg[:, 0:w], pb[:, 0:w], ALU.mult, [sgres, pbr], [sgres])
                            tt(acc[:, 0:w], acc[:, 0:w], sg[:, 0:w], ALU.add, [accres, sgres], [accres], e='pool')
                    yo, yores = yor.nxt()
                    cp(yo[:, 0:w], acc[:, 0:w], [accres], [yores], e='pool')
                    kb.dma('sp', YT[blk_of(u0)[0]][:, nch, 0:w], yo[:, 0:w], reads=[yores])
                pipeline(len(act_blocks), lda, cmpa)
            pipeline(KC, ldw, cmpw, depth=0)
        kb.barrier()
        if upto == 'merge':
            return False

        def units_wout(ph):
            return dict(gt=Rot(nc, ph, 'wo_g', [128, 512], F32, 2), xa=Rot(nc, ph, 'wo_x', [128, 512], F32, 3))

        def epi_wout(ct):
            def f(c):
                w = c['w']; si = c['si']
                if last and si == 1:
                    return
                gt, gtres = c['extra']['gt'].nxt()
                kb.dma('sp', gt[:], MODV[l, si, 2 * D + ct * 512: 2 * D + (ct + 1) * 512].partition_broadcast(128), writes=[gtres])
                for ti in range(w // 128):
                    rows = slice(c['u0'] + ti * 128, c['u0'] + (ti + 1) * 128)
                    xa, xares = c['extra']['xa'].nxt()
                    kb.dma('sp', xa[:], XA[rows, ct * 512:(ct + 1) * 512], writes=[xares])
                    p, pr = tmaj_unit(c, ti, 512, c['nk'])
                    o, orr = c['orot'].nxt()
                    tt(o[:], p[:, :], gt[:], ALU.mult, [pr, gtres], [orr])
                    stt(o[:], xa[:], ALPHA, o[:], ALU.mult, ALU.add, [xares, orr], [orr])
                    store(R1[rows, ct * 512:(ct + 1) * 512], o[:], orr)
            return f
        setbanks([0, 1, 2, 3, 4, 5])
        linear([dict(cols=[(ct * 512, 512)], epi=epi_wout(ct)) for ct in range(8)], lambda c0, n: w_out[l][:, c0:c0 + n], YT, KC, units_wout)
        if upto == 'wout':
            return False
        return PHASES_REST3(l, last, act_chunks, act_blocks)

    def setbanks(lst):
        bankset[0] = list(lst)
        psi[0] = 0

    def ln_rows(x, n, xres, g_t, b_t, out, outres, strot, junk, junkres):
        s, sr = strot.nxt()
        kb.op('dve', lambda: nc.vector.tensor_reduce(out=s[:, 0:1], in_=x, axis=AX.X, op=ALU.add), [xres], [sr])
        ts(s[:, 1:2], s[:, 0:1], -1.0 / n, None, ALU.mult, None, [sr], [sr])
        act(x, x, AF.Identity, [xres, sr], [xres], bias=s[:, 1:2])
        act(junk, x, AF.Square, [xres], [junkres, sr], accum_out=s[:, 2:3])
        act(s[:, 3:4], s[:, 2:3], AF.Sqrt, [sr], [sr], scale=1.0 / n, bias=EPSC[:, 0:1])
        kb.op('dve', lambda: nc.vector.reciprocal(out=s[:, 4:5], in_=s[:, 3:4]), [sr], [sr])
        stt(out, x, s[:, 4:5], g_t, ALU.mult, ALU.mult, [xres, sr, 'lnconst'], [outres])
        tt(out, out, b_t, ALU.add, [outres, 'lnconst'], [outres])

    def PHASES_REST(l, last, linear, fmaj_unit, tmaj_unit, store, gelu_tanh):
        act_chunks = [(u, si) for (u, si) in chunks if (si == 0 or not last)]
        act_blocks = [b for b in blocks if (b[2] == 0 or not last)]
        with ExitStack() as ph:
            setbanks([0, 1, 2, 3, 4, 5])
            lg = ph.enter_context(sbt(nc, 'g_lg', [128, BW], F32))
            lb = ph.enter_context(sbt(nc, 'g_lb', [128, BW], F32))
            bsb = ph.enter_context(sbt(nc, 'g_bs', [128, BW], F32))
            wst = ph.enter_context(sbt(nc, 'g_ws', [128, 8, 128], BF16))
            kb.dma('sp', lg[:], gm_ln_g[l].partition_broadcast(128), writes=['lnconst'])
            kb.dma('sp', lb[:], gm_ln_b[l].partition_broadcast(128), writes=['lnconst'])
            kb.dma('sp', bsb[:], gm_b_s[l].partition_broadcast(128), writes=['lnconst'])
            for g_ in range(8):
                kb.dma('pool', wst[:, g_, :], gm_wsT[l][g_], writes=['g_ws'])
            vr = Rot(nc, ph, 'gv', [128, BW], F32, 2)
            vn = Rot(nc, ph, 'gvn', [128, BW], BF16, 2)
            jk = Rot(nc, ph, 'gjk', [128, BW], BF16, 1)
            ur = Rot(nc, ph, 'gu', [128, 8, 128], BF16, 2)
            orr_ = Rot(nc, ph, 'go', [128, 8, 128], BF16, 2)
            tmp = Rot(nc, ph, 'gtmp', [128, BW], F32, 2)
            srot = Rot(nc, ph, 'gst', [128, 8], F32, 3)
            tl = {}

            def ld(i):
                u, si = act_chunks[i]
                v, vres = vr.nxt(); ut, ures = ur.nxt()
                tl[i] = (v, vres, ut, ures)
                kb.dma('sp', v[:], GV[u:u + 128, :], writes=[vres])
                kb.dma('sp', ut[:], GU.rearrange('(g c) t -> c g t', c=128)[:, :, u:u + 128], writes=[ures])

            def cmp_(i):
                u, si = act_chunks[i]
                v, vres, ut, ures = tl.pop(i)
                n_, nres = vn.nxt()
                j_, jres = jk.nxt()
                ln_rows(v[:], BW, vres, lg[:], lb[:], v[:], vres, srot, j_[:], jres)
                cp(n_[:], v[:], [vres], [nres], e='pool')
                o, ores = orr_.nxt()
                t_, tres = tmp.nxt()
                for hf in range(2):
                    p, pr = bank()
                    for g4 in range(4):
                        g = hf * 4 + g4
                        mm(p[:, g4 * 128:(g4 + 1) * 128], n_[:, g * 128:(g + 1) * 128], wst[:, g, :], True, True, [nres, 'g_ws'], [pr])
                    sl = slice(hf * 512, (hf + 1) * 512)
                    tt(t_[:, sl], p[:, :], bsb[:, sl], ALU.add, [pr, 'lnconst'], [tres])
                    tt(o[:].rearrange('c g p -> c (g p)')[:, sl], t_[:, sl], ut[:].rearrange('c g p -> c (g p)')[:, sl], ALU.mult, [tres, ures], [ores])
                bi_, bo_ = blk_of(u)
                kb.dma('sp', BR[0][bi_][:, :, bo_:bo_ + 128], o[:], reads=[ores])
            pipeline(len(act_chunks), ld, cmp_)
        kb.barrier()
        if upto == 'gmlp':
            return False

        with ExitStack() as ph:
            gq = ph.enter_context(sbt(nc, 'a_gq', [128, 128], F32))
            gk = ph.enter_context(sbt(nc, 'a_gk', [128, 128], F32))
            kb.dma('sp', gq[:], aq_gain[l].partition_broadcast(128), writes=['again'])
            kb.dma('sp', gk[:], ak_gain[l].partition_broadcast(128), writes=['again'])
            qr = Rot(nc, ph, 'aq', [128, 10, 128], F32, 2)
            cr = Rot(nc, ph, 'acs', [128, 2, 64], F32, 2)
            sqr = Rot(nc, ph, 'asq', [128, 10, 128], F32, 1)
            qn = Rot(nc, ph, 'aqn', [128, 10, 128], F32, 1)
            t1r = Rot(nc, ph, 'at1', [128, 10, 64], F32, 2)
            qf = Rot(nc, ph, 'aqf', [128, 10, 128], BF16, 2)
            qT = Rot(nc, ph, 'aqT', [128, 10, 128], BF16, 2)
            srot = Rot(nc, ph, 'ast', [128, 32], F32, 2)
            tl = {}

            def ld(i):
                u, si = chunks[i]
                q, qres = qr.nxt(); c_, cres = cr.nxt()
                tl[i] = (q, qres, c_, cres)
                kb.dma('sp', q[:, 0:8, :], AQ[u:u + 128, :].rearrange('t (h d) -> t h d', d=128), writes=[qres])
                kb.dma('sp', q[:, 8:10, :], AK[u:u + 128, :].rearrange('t (h d) -> t h d', d=128), writes=[qres])
                if si == 0:
                    kb.dma('sp', c_[:, 0, :], ropeC[u:u + 128, :], writes=[cres])
                    kb.dma('sp', c_[:, 1, :], ropeS[u:u + 128, :], writes=[cres])

            def cmp_(i):
                u, si = chunks[i]
                q, qres, c_, cres = tl.pop(i)
                sq, sqres = sqr.nxt()
                s, sres = srot.nxt()
                tt(sq[:], q[:], q[:], ALU.mult, [qres], [sqres])
                kb.op('dve', lambda: nc.vector.tensor_reduce(out=s[:, 0:10], in_=sq[:], axis=AX.X, op=ALU.add), [sqres], [sres])
                act(s[:, 10:20], s[:, 0:10], AF.Sqrt, [sres], [sres], scale=1.0 / 128, bias=EPSC[:, 0:1])
                kb.op('dve', lambda: nc.vector.reciprocal(out=s[:, 20:30], in_=s[:, 10:20]), [sres], [sres])
                n_, nres = qn.nxt()
                tt(n_[:], q[:], s[:, 20:30].unsqueeze(2).broadcast_to([128, 10, 128]), ALU.mult, [qres, sres], [nres])
                tt(n_[:, 0:8, :], n_[:, 0:8, :], gq[:].unsqueeze(1).broadcast_to([128, 8, 128]), ALU.mult, [nres, 'again'], [nres])
                tt(n_[:, 8:10, :], n_[:, 8:10, :], gk[:].unsqueeze(1).broadcast_to([128, 2, 128]), ALU.mult, [nres, 'again'], [nres])
                f_, fres = qf.nxt()
                if si == 0:
                    nv = n_[:].rearrange('p h (a b d) -> p h a b d', a=2, b=2)
                    fv = f_[:].rearrange('p h (a b d) -> p h a b d', a=2, b=2)
                    x1 = nv[:, :, :, 0, :]; x2 = nv[:, :, :, 1, :]
                    C = c_[:, 0, :].rearrange('p (a d) -> p a d', a=2).unsqueeze(1).broadcast_to([128, 10, 2, 32])
                    S = c_[:, 1, :].rearrange('p (a d) -> p a d', a=2).unsqueeze(1).broadcast_to([128, 10, 2, 32])
                    ta, tar = t1r.nxt(); tb, tbr = t1r.nxt()
                    tav = ta[:].rearrange('p h (a d) -> p h a d', a=2); tbv = tb[:].rearrange('p h (a d) -> p h a d', a=2)
                    tt(tav, x1, C, ALU.mult, [nres, cres], [tar])
                    tt(tbv, x2, S, ALU.mult, [nres, cres], [tbr])
                    tt(fv[:, :, :, 0, :], tav, tbv, ALU.subtract, [tar, tbr], [fres])
                    tt(tav, x2, C, ALU.mult, [nres, cres], [tar])
                    tt(tbv, x1, S, ALU.mult, [nres, cres], [tbr])
                    tt(fv[:, :, :, 1, :], tav, tbv, ALU.add, [tar, tbr], [fres])
                else:
                    cp(f_[:], n_[:], [nres], [fres])
                o, ores = qT.nxt()
                for hh in range(10):
                    b0 = (hh % 8) * 128
                    tr(PSB[:, b0:b0 + 128], f_[:, hh, :], IDB[:], [fres, 'IDB'], ['psb'])
                    if hh == 7:
                        cp(o[:, 0:8, :], PSB[:, 0:1024].rearrange('p (h t) -> p h t', t=128), ['psb'], [ores])
                cp(o[:, 8:10, :], PSB[:, 0:256].rearrange('p (h t) -> p h t', t=128), ['psb'], [ores])
                kb.dma('sp', QT.rearrange('(h d) t -> d h t', d=128)[:, :, u:u + 128], o[:, 0:8, :], reads=[ores])
                kb.dma('sp', KT.rearrange('(h d) t -> d h t', d=128)[:, :, u:u + 128], o[:, 8:10, :], reads=[ores])
            pipeline(len(chunks), ld, cmp_)
        kb.barrier()

        with ExitStack() as ph:
            kts = ph.enter_context(sbt(nc, 'at_k', [128, 2, T], BF16))
            vs = ph.enter_context(sbt(nc, 'at_v', [128, NCH, 256], BF16))
            kb.dma('sp', kts[:], KT.rearrange('(g d) t -> d g t', d=128), writes=['at_k'])
            for n0_ in range(0, NCH, 8):
                n1_ = min(NCH, n0_ + 8)
                kb.dma('sp', vs[:, n0_:n1_, :], AV[n0_ * 128:n1_ * 128, :].rearrange('(n p) c -> p n c', p=128), writes=['at_v'])
            qb = Rot(nc, ph, 'at_q', [128, 512], BF16, 3)
            ptr_ = Rot(nc, ph, 'at_p', [128, 512], BF16, 3)
            rcr = Rot(nc, ph, 'at_rc', [128, 512], F32, 2)
            obr = Rot(nc, ph, 'at_o', [128, 512], BF16, 2)
            setbanks([0, 1, 2])
            work = []
            for (u0, w, si) in act_blocks:
                keych = list(range(NCH)) if si == 0 else list(range(SEQ // 128, NCH))
                for h in range(8):
                    work.append((u0, w, si, h, keych))
            tl = {}

            def ld(i):
                u0, w, si, h, keych = work[i]
                q, qres = qb.nxt(); tl[i] = (q, qres)
                kb.dma('sp', q[:, 0:w], QT[h * 128:(h + 1) * 128, u0:u0 + w], writes=[qres])

            def cmp_(i):
                u0, w, si, h, keych = work[i]
                q, qres = tl.pop(i)
                g = h // 4
                PO, POr = PS[3 + (i % 2)], ('ps', 3 + (i % 2))
                PM, PMr = PS[5 + (i % 2)], ('ps', 5 + (i % 2))
                for ki, n in enumerate(keych):
                    p, pr = bank()
                    mm(p[:, 0:w], kts[:, g, n * 128:(n + 1) * 128], q[:, 0:w], True, True, ['at_k', qres], [pr])
                    pt, ptres = ptr_.nxt()
                    act(pt[:, 0:w], p[:, 0:w], AF.Exp, [pr], [ptres], scale=float(128 ** -0.5))
                    mm(PO[:, 0:w], vs[:, n, g * 128:(g + 1) * 128], pt[:, 0:w], ki == 0, ki == len(keych) - 1, ['at_v', ptres], [POr])
                    mm(PM[:, 0:w], ONEB[:], pt[:, 0:w], ki == 0, ki == len(keych) - 1, ['ONEB', ptres], [PMr])
                rc, rcres = rcr.nxt()
                kb.op('dve', lambda: nc.vector.reciprocal(out=rc[:, 0:w], in_=PM[:, 0:w]), [PMr], [rcres])
                o, ores = obr.nxt()
                tt(o[:, 0:w], PO[:, 0:w], rc[:, 0:w], ALU.mult, [POr, rcres], [ores])
                kb.dma('sp', BR[1][blk_of(u0)[0]][:, h, 0:w], o[:, 0:w], reads=[ores])
            pipeline(len(work), ld, cmp_)
        kb.barrier()
        if upto == 'att':
            return False

        with ExitStack() as ph:
            wT = ph.enter_context(sbt(nc, 'c_w', [128, 8, 31], F32))
            cb = ph.enter_context(sbt(nc, 'c_b', [128, 8], F32))
            cg_ = ph.enter_context(sbt(nc, 'c_g', [128, 8], F32))
            cbb = ph.enter_context(sbt(nc, 'c_bb', [128, 8], F32))
            kb.dma('sp', wT[:], conv_wT[l].rearrange('(cc c) j -> c cc j', c=128), writes=['cconst'])
            kb.dma('sp', cb[:], conv_bc[l], writes=['cconst'])
            kb.dma('sp', cg_[:], conv_ln_gc[l], writes=['cconst'])
            kb.dma('sp', cbb[:], conv_ln_bc[l], writes=['cconst'])
            gin = Rot(nc, ph, 'c_in', [128, 8, 542], F32, 2)
            Y = ph.enter_context(sbt(nc, 'c_y', [128, 8, 512], F32))
            Y2 = ph.enter_context(sbt(nc, 'c_y2', [128, 8, 512], F32))
            mt = Rot(nc, ph, 'c_m', [128, 4, 512], F32, 1)
            zr = Rot(nc, ph, 'c_z', [128, 512], F32, 2)
            zo = Rot(nc, ph, 'c_zo', [128, 512], BF16, 2)
            srng = {0: (0, SEQ), 1: (SEQ, T)}
            tl = {}

            def ld(i):
                u0, w, si = act_blocks[i]
                g_, gres = gin.nxt(); tl[i] = (g_, gres)
                s0, s1 = srng[si]
                a = max(s0, u0 - 15); b = min(s1, u0 + w + 15)
                kb.op('pool', lambda: nc.gpsimd.memset(g_[:], 0.0), [], [gres])
                off = a - (u0 - 15)
                kb.dma('sp', g_[:, :, off:off + (b - a)], CG.rearrange('(cc c) t -> c cc t', c=128)[:, :, a:b], writes=[gres])

            def cmp_(i):
                u0, w, si = act_blocks[i]
                g_, gres = tl.pop(i)
                for cc in range(8):
                    ts(Y[:, cc, 0:w], g_[:, cc, 0:w], wT[:, cc, 0:1], cb[:, cc:cc + 1], ALU.mult, ALU.add, [gres, 'cconst'], [('cy', cc)])
                    for j in range(1, 31):
                        stt(Y[:, cc, 0:w], g_[:, cc, j:j + w], wT[:, cc, j:j + 1], Y[:, cc, 0:w], ALU.mult, ALU.add, [gres, 'cconst', ('cy', cc)], [('cy', cc)])
                    act(Y2[:, cc, 0:w], Y[:, cc, 0:w], AF.Square, [('cy', cc)], [('cy2', cc)])
                S1, S1r = PS[0], ('ps', 0)
                S2, S2r = PS[1], ('ps', 1)
                for cc in range(8):
                    mm(S1[:, 0:w], ONEF, Y[:, cc, 0:w], cc == 0, cc == 7, ['CST', ('cy', cc)], [S1r])
                for cc in range(8):
                    mm(S2[:, 0:w], ONEF, Y2[:, cc, 0:w], cc == 0, cc == 7, ['CST', ('cy2', cc)], [S2r])
                m_, mres = mt.nxt()
                ts(m_[:, 0, 0:w], S1[:, 0:w], 1.0 / BW, None, ALU.mult, None, [S1r], [mres])
                tt(m_[:, 1, 0:w], m_[:, 0, 0:w], m_[:, 0, 0:w], ALU.mult, [mres], [mres])
                stt(m_[:, 2, 0:w], S2[:, 0:w], 1.0 / BW, m_[:, 1, 0:w], ALU.mult, ALU.subtract, [S2r, mres], [mres])
                act(m_[:, 2, 0:w], m_[:, 2, 0:w], AF.Sqrt, [mres], [mres], bias=EPSC[:, 0:1])
                kb.op('dve', lambda: nc.vector.reciprocal(out=m_[:, 3, 0:w], in_=m_[:, 2, 0:w]), [mres], [mres])
                for cc in range(8):
                    z, zres = zr.nxt()
                    tt(z[:, 0:w], Y[:, cc, 0:w], m_[:, 0, 0:w], ALU.subtract, [('cy', cc), mres], [zres])
                    tt(z[:, 0:w], z[:, 0:w], m_[:, 3, 0:w], ALU.mult, [zres, mres], [zres])
                    o, ores = zo.nxt()
                    act(z[:, 0:w], z[:, 0:w], AF.Identity, [zres, 'cconst'], [zres], scale=cg_[:, cc:cc + 1], bias=cbb[:, cc:cc + 1])
                    act(o[:, 0:w], z[:, 0:w], AF.Silu, [zres], [ores])
                    kb.dma('sp', BR[2][blk_of(u0)[0]][:, cc, 0:w], o[:, 0:w], reads=[ores])
            pipeline(len(act_blocks), ld, cmp_)
        kb.barrier()
        if upto == 'conv':
            return False
        return PHASES_REST2(l, last, linear, fmaj_unit, tmaj_unit, store, act_chunks, act_blocks)


    ok = True
    for l in range(L):
        if not layer(l):
            ok = False
            break
    if ok:
        for r0_ in range(0, SEQ, 512):
            kb.dma('sp', out_d[r0_:r0_ + 512, :], XA[r0_:r0_ + 512, :], key='fin')
    kb.barrier()
    st0.close()
    build.ninst = kb.ninst
    return nc


def prep_inputs(inp, SEQ, CTX, L):
    f = np.float32
    d = {}
    d['x'] = np.ascontiguousarray(inp['x'].reshape(SEQ, D), f)
    d['ctx'] = np.ascontiguousarray(inp['ctx'].reshape(CTX, D), f)
    cc = np.stack([inp['c'].reshape(D), inp['c_ctx'].reshape(D)], axis=-1)
    d['cc'] = np.ascontiguousarray(cc.reshape(KC, 128, 2).transpose(1, 0, 2), f)
    d['w_mod'] = inp['w_mod'][:L]
    d['b_modc'] = np.ascontiguousarray(inp['b_mod'][:L].reshape(L, 192, 128).transpose(0, 2, 1), f)
    d['w_in'] = inp['w_in'][:L]
    for k in ['att_q_gain', 'att_k_gain', 'gm_ln_g', 'gm_ln_b', 'w_branch', 'w_out', 'ln1_g', 'ln1_b',
              'w_router', 'w_gate', 'w_up', 'w_down', 'ln2_g', 'ln2_b']:
        d[k] = inp[k][:L]
    d['gm_wsT'] = np.ascontiguousarray(inp['gm_w_s'][:L].transpose(0, 1, 3, 2), f)
    d['gm_b_s'] = np.ascontiguousarray(inp['gm_b_s'][:L].reshape(L, 1024), f)
    d['conv_wT'] = np.ascontiguousarray(inp['conv_w'][:L].transpose(0, 2, 1), f)
    for k, kk in [('conv_b', 'conv_bc'), ('conv_ln_g', 'conv_ln_gc'), ('conv_ln_b', 'conv_ln_bc'), ('ml_norm_g', 'ml_norm_gc')]:
        d[kk] = np.ascontiguousarray(inp[k][:L].reshape(L, 8, 128).transpose(0, 2, 1), f)
    d['ml_gate_bias'] = np.ascontiguousarray(inp['ml_gate_bias'][:L].reshape(L, 32), f)
    cst = np.zeros((128, 8, 128), f)
    i = np.arange(128)
    cst[:, 0] = np.eye(128)
    cst[:, 1] = (i[:, None] <= i[None, :])
    cst[:, 2] = (i[:, None] >= i[None, :])
    cst[:, 3] = 1.0
    cst[:, 4] = (i[:, None] <= i[None, :])
    cst[:, 5] = (i[:, None] >= i[None, :])
    cst[:, 6] = (i[:, None] < i[None, :])
    d['cst'] = cst
    t = np.arange(SEQ)
    row = (t // 64).astype(np.float64); col = (t % 64).astype(np.float64)
    inv = 10000.0 ** (-np.arange(32, dtype=np.float64) / 32)
    ar = (row[:, None].astype(f) * inv[None, :].astype(f)).astype(f)
    ac = (col[:, None].astype(f) * inv[None, :].astype(f)).astype(f)
    d['ropeC'] = np.concatenate([np.cos(ar), np.cos(ac)], axis=1).astype(f)
    d['ropeS'] = np.concatenate([np.sin(ar), np.sin(ac)], axis=1).astype(f)
    d['iota'] = np.ascontiguousarray(np.broadcast_to(np.arange(1024, dtype=f)[None, :], (128, 1024)))
    d['tid'] = (np.arange(64)[None, :] * 128 + np.arange(128)[:, None]).astype(f)
    d['zeros'] = np.zeros((128, 4096), f)
    return d


_CACHE = {}


def kernel(**inputs):
    SEQ, CTX, L = 8192, 256, 2
    if 'nc' not in _CACHE:
        _CACHE['nc'] = build(SEQ, CTX, L)
    d = prep_inputs(inputs, SEQ, CTX, L)
    res = run_bass_kernel_spmd(_CACHE['nc'], [d], core_ids=[0])
    return np.asarray(res.results[0]['out'], np.float32).reshape(1, SEQ, D)
```
